# Optimizing a Trainium2 kernel written in Bass

```python
import math
import jax, jax.numpy as jnp
from jax import lax
import numpy as np

D_MODEL = 1024
BATCH = 8
SEQ = 4096
DEPTH = 1

D_MIX = D_MODEL
M_HEADS = 4
M_HEAD_DIM = 128
M_WIDTH = M_HEADS * M_HEAD_DIM
M_CHUNK = 64
M_CONV = 4
M_INIT = -1e30
A_HEADS = 8
A_KV_HEADS = 2
A_HEAD_DIM = 64
A_WIDTH = A_HEADS * A_HEAD_DIM
A_GROUP = A_HEADS // A_KV_HEADS
WINDOW = 128
ROPE_THETA = 500000.0
ROT_DIM = A_HEAD_DIM // 4
D_FF = int(math.ceil(8 * D_MODEL / 3 / 256) * 256)
EPS = 1e-6

IN_SIZES = (M_WIDTH, M_WIDTH, M_WIDTH, M_WIDTH, M_HEADS, M_HEADS,
            A_WIDTH, A_KV_HEADS * A_HEAD_DIM, A_KV_HEADS * A_HEAD_DIM)
D_IN = sum(IN_SIZES)

kernel_name = "hybrid_mlstm_swa_sink_swiglu"


def _rmsnorm(x, g):
    xf = x.astype(jnp.float32)
    y = xf * lax.rsqrt(jnp.mean(xf * xf, axis=-1, keepdims=True) + EPS)
    return (y * g.astype(jnp.float32)).astype(x.dtype)


def _causal_conv_silu(x, w):
    k = w.shape[0]
    y = lax.conv_general_dilated(x, w[:, None, :].astype(x.dtype), window_strides=(1,),
                                 padding=((k - 1, 0),),
                                 dimension_numbers=("NWC", "WIO", "NWC"),
                                 feature_group_count=x.shape[-1])
    return jax.nn.silu(y)


def _rope_partial(x, cos, sin):
    xr, xp = x[..., :ROT_DIM], x[..., ROT_DIM:]
    xr = xr.astype(jnp.float32)
    x1, x2 = xr[..., :ROT_DIM // 2], xr[..., ROT_DIM // 2:]
    c = cos[None, :, None, :]
    s = sin[None, :, None, :]
    rot = jnp.concatenate([x1 * c - x2 * s, x2 * c + x1 * s], axis=-1)
    return jnp.concatenate([rot.astype(x.dtype), xp], axis=-1)


def _mlstm(q, k, v, i_pre, f_pre):
    f32 = jnp.float32
    B, S, NH, DH = q.shape
    L = M_CHUNK
    NC = S // L

    def chunk4(t):
        return t.astype(f32).reshape(B, NC, L, NH, DH).transpose(0, 3, 1, 2, 4)

    def chunk3(t):
        return t.astype(f32).reshape(B, NC, L, NH).transpose(0, 3, 1, 2)

    q = chunk4(q)
    k = chunk4(k) * (DH ** -0.5)
    v = chunk4(v)
    ig = chunk3(i_pre)
    lf = jax.nn.log_sigmoid(chunk3(f_pre))
    b = jnp.cumsum(lf, axis=-1)
    g = b[..., -1]

    a = g[..., None] - b + ig
    m_loc = jnp.max(a, axis=-1)
    w = jnp.exp(a - m_loc[..., None])
    dC = jnp.einsum('bhcl,bhcld,bhcle->bhcde', w, k, v)
    dn = jnp.einsum('bhcl,bhcld->bhcd', w, k)

    def step(carry, inp):
        C, n, m = carry
        dC_c, dn_c, g_c, ml_c = inp
        m_new = jnp.maximum(g_c + m, ml_c)
        s_old = jnp.exp(g_c + m - m_new)
        s_new = jnp.exp(ml_c - m_new)
        C_new = s_old[..., None, None] * C + s_new[..., None, None] * dC_c
        n_new = s_old[..., None] * n + s_new[..., None] * dn_c
        return (C_new, n_new, m_new), (C, n, m)

    init = (jnp.zeros((B, NH, DH, DH), f32), jnp.zeros((B, NH, DH), f32),
            jnp.full((B, NH), M_INIT, f32))
    xs = (jnp.moveaxis(dC, 2, 0), jnp.moveaxis(dn, 2, 0), jnp.moveaxis(g, 2, 0), jnp.moveaxis(m_loc, 2, 0))
    _, (Cs, ns, ms) = lax.scan(step, init, xs)
    Cs = jnp.moveaxis(Cs, 0, 2)
    ns = jnp.moveaxis(ns, 0, 2)
    ms = jnp.moveaxis(ms, 0, 2)

    causal = jnp.tril(jnp.ones((L, L), dtype=bool))
    D = jnp.where(causal, b[..., :, None] - b[..., None, :] + ig[..., None, :], -jnp.inf)
    inter = b + ms[..., None]
    m_t = jnp.maximum(jnp.max(D, axis=-1), inter)
    Sw = jnp.einsum('bhcld,bhcsd->bhcls', q, k) * jnp.exp(D - m_t[..., None])
    sc = jnp.exp(inter - m_t)
    num = jnp.einsum('bhcls,bhcse->bhcle', Sw, v) + sc[..., None] * jnp.einsum('bhcld,bhcde->bhcle', q, Cs)
    den = jnp.sum(Sw, axis=-1) + sc * jnp.einsum('bhcld,bhcd->bhcl', q, ns)
    h = num / jnp.maximum(jnp.abs(den), jnp.exp(-m_t))[..., None]
    return h.transpose(0, 2, 3, 1, 4).reshape(B, S, NH, DH)


def _swa_sinks(q, k, v, sinks):
    B, S, HQ, Dh = q.shape
    W = WINDOW
    NB = S // W
    qb = q.reshape(B, NB, W, A_KV_HEADS, A_GROUP, Dh)
    kb = k.reshape(B, NB, W, A_KV_HEADS, Dh)
    vb = v.reshape(B, NB, W, A_KV_HEADS, Dh)
    pad = jnp.zeros_like(kb[:, :1])
    k2 = jnp.concatenate([jnp.concatenate([pad, kb[:, :-1]], axis=1), kb], axis=2)
    v2 = jnp.concatenate([jnp.concatenate([pad, vb[:, :-1]], axis=1), vb], axis=2)
    s = jnp.einsum('bnqhgd,bnkhd->bnhgqk', qb, k2).astype(jnp.float32) * (Dh ** -0.5)
    qpos = jnp.arange(W)[:, None] + W
    kpos = jnp.arange(2 * W)[None, :]
    rel = qpos - kpos
    band = (rel >= 0) & (rel < W)
    valid = band[None] & ((jnp.arange(NB)[:, None, None] > 0) | (kpos >= W)[None])
    s = jnp.where(valid[None, :, None, None], s, -jnp.inf)
    sink = sinks.astype(jnp.float32).reshape(1, 1, A_KV_HEADS, A_GROUP, 1, 1)
    logits = jnp.concatenate([s, jnp.broadcast_to(sink, s.shape[:-1] + (1,))], axis=-1)
    p = jax.nn.softmax(logits, axis=-1)[..., :-1]
    o = jnp.einsum('bnhgqk,bnkhd->bnqhgd', p.astype(v.dtype), v2)
    return o.reshape(B, S, HQ, Dh)


def setup_inputs(seed: int = 0) -> dict:
    key = jax.random.key(seed)
    ks = jax.random.split(key, 16)
    f32 = jnp.float32
    nrm = lambda k_, shp, sc: (jax.random.normal(k_, shp, f32) * sc)
    return {
        "x": jax.random.normal(ks[0], (BATCH, SEQ, D_MODEL), f32),
        "norm1_g": 1.0 + nrm(ks[1], (DEPTH, D_MODEL), 0.02),
        "w_in": nrm(ks[2], (DEPTH, D_MODEL, D_IN), D_MODEL ** -0.5),
        "conv_w": nrm(ks[3], (DEPTH, M_CONV, 2 * M_WIDTH), M_CONV ** -0.5),
        "igate_b": nrm(ks[4], (DEPTH, M_HEADS), 0.1),
        "fgate_b": jax.random.uniform(ks[5], (DEPTH, M_HEADS), f32, 3.0, 6.0),
        "mlstm_norm_g": 1.0 + nrm(ks[6], (DEPTH, M_WIDTH), 0.02),
        "q_norm_g": 1.0 + nrm(ks[7], (DEPTH, A_HEAD_DIM), 0.02),
        "k_norm_g": 1.0 + nrm(ks[8], (DEPTH, A_HEAD_DIM), 0.02),
        "sinks": nrm(ks[9], (DEPTH, A_HEADS), 1.0),
        "w_out": nrm(ks[10], (DEPTH, D_MIX, D_MODEL), D_MIX ** -0.5),
        "norm2_g": 1.0 + nrm(ks[11], (DEPTH, D_MODEL), 0.02),
        "w_gate": nrm(ks[12], (DEPTH, D_MODEL, D_FF), D_MODEL ** -0.5),
        "w_up": nrm(ks[13], (DEPTH, D_MODEL, D_FF), D_MODEL ** -0.5),
        "w_down": nrm(ks[14], (DEPTH, D_FF, D_MODEL), D_FF ** -0.5),
    }


def reference(x, norm1_g, w_in, conv_w, igate_b, fgate_b, mlstm_norm_g, q_norm_g, k_norm_g,
              sinks, w_out, norm2_g, w_gate, w_up, w_down):
    B, S, _ = x.shape
    f32 = jnp.float32
    pos = jnp.arange(S, dtype=f32)
    inv_freq = ROPE_THETA ** (-jnp.arange(0, ROT_DIM, 2, dtype=f32) / ROT_DIM)
    ang = pos[:, None] * inv_freq[None, :]
    cos, sin = jnp.cos(ang), jnp.sin(ang)
    split_at = [int(i) for i in np.cumsum(IN_SIZES)[:-1]]

    h = x
    for l in range(DEPTH):
        u = _rmsnorm(h, norm1_g[l])
        proj = u @ w_in[l]
        mq, mk, mv, mo, mi, mf, aq, ak, av = jnp.split(proj, split_at, axis=-1)

        qk = _causal_conv_silu(jnp.concatenate([mq, mk], axis=-1), conv_w[l])
        mq_c, mk_c = qk[..., :M_WIDTH], qk[..., M_WIDTH:]
        hm = _mlstm(mq_c.reshape(B, S, M_HEADS, M_HEAD_DIM),
                    mk_c.reshape(B, S, M_HEADS, M_HEAD_DIM),
                    mv.reshape(B, S, M_HEADS, M_HEAD_DIM),
                    mi.astype(f32) + igate_b[l].astype(f32),
                    mf.astype(f32) + fgate_b[l].astype(f32))
        hm = _rmsnorm(hm, mlstm_norm_g[l].reshape(M_HEADS, M_HEAD_DIM))
        hm = (hm * jax.nn.sigmoid(mo.astype(f32)).reshape(B, S, M_HEADS, M_HEAD_DIM))
        hm = hm.reshape(B, S, M_WIDTH).astype(x.dtype)

        q = _rope_partial(_rmsnorm(aq.reshape(B, S, A_HEADS, A_HEAD_DIM), q_norm_g[l]), cos, sin)
        k = _rope_partial(_rmsnorm(ak.reshape(B, S, A_KV_HEADS, A_HEAD_DIM), k_norm_g[l]), cos, sin)
        v = av.reshape(B, S, A_KV_HEADS, A_HEAD_DIM)
        ha = _swa_sinks(q, k, v, sinks[l]).reshape(B, S, A_WIDTH).astype(x.dtype)

        h = h + jnp.concatenate([hm, ha], axis=-1) @ w_out[l]

        u2 = _rmsnorm(h, norm2_g[l])
        h = h + (jax.nn.silu(u2 @ w_gate[l]) * (u2 @ w_up[l])) @ w_down[l]
    return h
```

```python
import math
import sys
from contextlib import ExitStack

import numpy as np
import ml_dtypes

import concourse.bass as bass
import concourse.mybir as mybir
from concourse.bass_utils import run_bass_kernel_spmd

F32 = mybir.dt.float32
BF = mybir.dt.bfloat16
ALU = mybir.AluOpType
AF = mybir.ActivationFunctionType
AX = mybir.AxisListType

D_MODEL = 1024
D_FF = 2816
NFF = D_FF // 128
EPS = 1e-6
N_TM = 1800
NPAR = 1168


class Buf:
    __slots__ = ("name", "w", "r", "excl", "dsem", "dcount")

    def __init__(self, name, excl=False):
        self.name = name
        self.w = None
        self.r = {}
        self.excl = excl
        self.dsem = None
        self.dcount = 0


class Eng:
    def __init__(self, name, eng, sem, nowait_self=False):
        self.name = name
        self.eng = eng
        self.sem = sem
        self.n = 0
        self.ops = []
        self.waited = {}
        self.nowait_self = nowait_self


class Prog:
    def __init__(self, nc, stack):
        self.nc = nc
        self.stack = stack
        mk = lambda n: stack.enter_context(nc.semaphore(n))
        self.pe = Eng("pe", nc.tensor, mk("s_pe"), nowait_self=True)
        self.act = Eng("act", nc.scalar, mk("s_act"))
        self.dve = Eng("dve", nc.vector, mk("s_dve"))
        self.pool = Eng("pool", nc.gpsimd, mk("s_pool"))
        self.sp = Eng("sp", nc.sync, mk("s_sp"))

    def new_sem(self, name):
        return self.stack.enter_context(self.nc.semaphore(name))

    def _deps(self, E, reads, writes):
        toks = {}

        def add(t):
            if t is None:
                return
            s, v = t
            k = id(s)
            if k not in toks or toks[k][1] < v:
                toks[k] = (s, v)
        for b in reads:
            add(b.w)
        for b in writes:
            add(b.w)
            for t in b.r.values():
                add(t)
        out = []
        for k, (s, v) in toks.items():
            if E.nowait_self and s is E.sem:
                continue
            if E.waited.get(k, 0) >= v:
                continue
            E.waited[k] = v
            out.append((s, v))
        return out

    def _commit(self, tok, reads, writes):
        for b in writes:
            b.w = tok
            b.r = {}
        for b in reads:
            if b not in writes:
                k = id(tok[0])
                b.r[k] = tok

    def op(self, E, fn, reads=(), writes=()):
        reads = list(reads)
        writes = list(writes)
        for b in list(reads):
            if b.excl and b not in writes:
                writes.append(b)
        waits = self._deps(E, reads, writes)
        E.n += 1
        tok = (E.sem, E.n)
        E.ops.append((waits, fn, E.sem, 1, sys._getframe(1).f_lineno))
        self._commit(tok, reads, writes)
        return tok

    def dma(self, Q, fn, target, reads=(), writes=(), deps=True):
        waits = self._deps(Q, reads, writes) if deps else []
        if target.dsem is None:
            target.dsem = {}
        if Q.name not in target.dsem:
            target.dsem[Q.name] = [self.new_sem("d_%s_%s" % (target.name, Q.name)), 0]
        ent = target.dsem[Q.name]
        ent[1] += 16
        tok = (ent[0], ent[1])
        Q.ops.append((waits, fn, ent[0], 16, sys._getframe(1).f_lineno))
        self._commit(tok, reads, writes)
        return tok

    def wait_all(self, E, bufs):
        waits = self._deps(E, [], bufs)
        E.ops.append((waits, None, None, 0, 0))

    def emit(self):
        with self.nc.Block() as block:
            def run(E):
                def body(eng):
                    for waits, fn, sem, inc, ln in E.ops:
                        for (s, v) in waits:
                            eng.wait_ge(s, v)
                        if fn is not None:
                            fn(eng).then_inc(sem, inc).annotate("L%d" % ln)
                return body
            block.tensor(run(self.pe))
            block.scalar(run(self.act))
            block.vector(run(self.dve))
            block.gpsimd(run(self.pool))
            block.sync(run(self.sp))


class Tl:
    def __init__(self, t, b):
        self.t = t
        self.b = b

    def __getitem__(self, k):
        return self.t[k]


def mk(name, *a, **kw):
    return lambda e: getattr(e, name)(*a, **kw)


def build_nc(NT):
    T = 512
    NM = NT // T
    NTILE = NT // 128
    nc = bass.Bass("TRN2", target_bir_lowering=False)
    dt_in = lambda n, s, d=F32: nc.dram_tensor(n, s, d, kind="ExternalInput").ap()
    x_d = dt_in("x", [NT, D_MODEL])
    wtm_d = dt_in("w_in_tm", [D_MODEL, N_TM])
    wfm_d = dt_in("w_in_fm", [D_MODEL, 1024])
    wo_d = dt_in("w_out", [D_MODEL, D_MODEL])
    wg_d = dt_in("w_gate", [D_MODEL, D_FF])
    wu_d = dt_in("w_up", [D_MODEL, D_FF])
    wd_d = dt_in("w_down", [D_FF, D_MODEL])
    par_d = dt_in("params", [1, NPAR])
    g12_d = dt_in("g12", [128, 16])
    cw_d = dt_in("convw", [128, 32])
    cs_d = dt_in("ropecs", [128, NTILE * 16])
    id_d = dt_in("ident", [128, 128], BF)
    mk_d = dt_in("masks", [128, 256], BF)
    nm_d = dt_in("negmask", [128, 256], BF)
    tri_d = dt_in("tri", [128, 256])
    out_d = nc.dram_tensor("out", [NT, D_MODEL], F32, kind="ExternalOutput").ap()
    scfm_d = nc.dram_tensor("sc_fm", [8, 128, 1024], BF).ap()
    scg_d = nc.dram_tensor("sc_g", [NFF, 128, 1024], BF).ap()
    scu_d = nc.dram_tensor("sc_u", [NFF, 128, 1024], BF).ap()
    scd_d = nc.dram_tensor("sc_d", [D_FF, D_MODEL], BF).ap()

    with ExitStack() as st:
        P = Prog(nc, st)
        pe, act, dve, pool, sp = P.pe, P.act, P.dve, P.pool, P.sp

        def sb(name, shape, dt, nb=1):
            t = st.enter_context(nc.sbuf_tensor("sb_" + name, shape, dt))
            return Tl(t, Buf(name) if nb == 1 else [Buf(f"{name}{i}") for i in range(nb)])

        banks = [Tl(st.enter_context(nc.psum_tensor(f"bank{i}", [128, 512], F32)), Buf(f"bank{i}", excl=True))
                 for i in range(8)]
        bank_ctr = [0]

        def nb_():
            b = banks[bank_ctr[0] % 4]
            bank_ctr[0] += 1
            return b
        fbank_ctr = [0]

        def nbf_():
            b = banks[4 + fbank_ctr[0] % 4]
            fbank_ctr[0] += 1
            return b

        WIT = sb("WIT", [128, 8, N_TM], BF)
        WO = sb("WO", [128, 8, D_MODEL], BF)
        xs = [[sb(f"x{i}_{s}", [128, D_MODEL], F32) for s in range(4)] for i in range(2)]
        ubf = [sb(f"ubf{i}", [128, D_MODEL], BF) for i in range(2)]
        uT = sb("uT", [128, 8, T], BF, nb=4)
        u2T = sb("u2T", [128, 8, T], BF, nb=4)
        pre = [sb(f"pre{i}", [128, T + 3], F32) for i in range(2)]
        yc = [sb(f"yc{i}", [128, T], F32) for i in range(2)]
        halo = sb("halo", [128, 8, 3], F32, nb=8)
        qkT = sb("qkT", [128, 8, T], BF, nb=8)
        hcatT = sb("hcatT", [128, 8, T], BF, nb=4)
        actT = sb("actT", [128, NFF, T], BF, nb=NFF)
        sg = [sb(f"sg{i}", [128, T], BF) for i in range(2)]
        NR = 4
        ring = [sb(f"ring{i}", [128, 8, 128], BF) for i in range(NR)]
        ND = 6
        dring = [sb(f"dring{i}", [128, 2, 512], BF) for i in range(ND)]
        fring = [sb(f"fring{i}", [128, 8, 128], BF) for i in range(2)]
        ident = sb("ident", [128, 128], BF)
        masks = sb("masks", [128, 256], BF)
        negm = sb("negm", [128, 256], BF)
        tri = sb("tri", [128, 256], F32)
        par = sb("par", [128, NPAR], F32)
        g12 = sb("g12", [128, 16], F32)
        cw = sb("cw", [128, 32], F32)
        cs = sb("cs", [128, NTILE * 16], F32)
        esink = sb("esink", [128, 8], F32)
        ssq = sb("ssq", [128, 1], F32)
        rstd = sb("rstd", [128, 1], F32)
        v_sb = sb("v_sb", [128, 512], BF)
        vt = sb("vt", [128, 4, 129], BF)
        og = sb("og", [128, 512], F32)
        qk_sb = sb("qk_sb", [128, 640], F32)
        sq = sb("sq", [128, 640], F32)
        qkn = sb("qkn", [128, 640], BF)
        ssq10 = sb("ssq10", [128, 10], F32)
        r10 = sb("r10", [128, 10], F32)
        rt = sb("rt", [128, 4, 80], F32)
        QT2 = [sb(f"QT{i}", [128, 4, 128], BF) for i in range(2)]
        KT = [sb(f"KT{i}", [128, 128], BF) for i in range(3)]
        Vaug = [sb(f"Vaug{i}", [128, 2, 65], BF) for i in range(3)]
        PT = sb("PT", [128, 4, 512], BF, nb=4)
        Sm = sb("Sm", [128, 4, 128], BF)
        k_tm = sb("k_tm", [128, 512], BF)
        hmf = sb("hmf", [128, 512], F32)
        hm = sb("hm", [128, 512], BF)
        ha = sb("ha", [128, 512], BF)
        C32 = sb("C32", [128, 4, 129], F32)
        Cbf = sb("Cbf", [128, 4, 129], BF)
        gsb4 = sb("gsb4", [128, 32], F32)
        l14 = sb("l14", [128, 16], F32)
        gq4 = sb("gq4", [128, 32], F32)
        apr4 = sb("apr4", [128, 16], F32)
        E4 = sb("E4", [128, 16], F32)
        XG = sb("XG", [128, 32], F32)
        S4 = sb("S4", [128, 16], F32)
        W3 = sb("W3", [128, 4, 12], F32, nb=4)
        Mst = sb("Mst", [128, 4], F32)
        Mx = sb("Mx", [128, 4], F32)
        inv4 = sb("inv4", [128, 4], F32)
        neghalf = sb("neghalf", [128, 16], F32)
        l1 = sb("l1", [128, 4], F32)
        gq = sb("gq", [128, 8], F32)
        apr = sb("apr", [128, 4], F32)
        Eex = sb("Eex", [128, 4], F32)
        Rl = sb("Rl", [128, 4], F32)
        Rr = sb("Rr", [128, 4], F32)
        ms = sb("ms", [128, 4], F32)
        D3 = sb("D3", [128, 12], F32)
        X3 = sb("X3", [128, 12], F32)
        dd = sb("dd", [128, 4], F32)
        rd = sb("rd", [128, 4], F32)
        ss4 = sb("ss4", [128, 4], F32)
        t4 = sb("t4", [128, 4], F32)
        sc4 = sb("sc4", [128, 4], F32)
        den8 = sb("den8", [128, 8], F32)
        rden8 = sb("rden8", [128, 8], F32)

        b_scfm, b_scg, b_scu, b_scd = Buf("scfm"), Buf("scg"), Buf("scu"), Buf("scd")
        b_out = Buf("out")
        b_c = Buf("consts")

        gbias = lambda: par[:, 0:8]
        sinks_ap = lambda: par[:, 8:16]
        gqk_ap = lambda: par[:, 16:656]
        gm_ap = lambda: par[:, 656:1168]

        def cload(dst, src):
            P.dma(sp, mk("dma_start", out=dst, in_=src), b_c, writes=[b_c], deps=False)
        cload(ident[:], id_d)
        cload(masks[:], mk_d)
        cload(negm[:], nm_d)
        cload(tri[:], tri_d)
        cload(g12[:], g12_d)
        cload(cw[:], cw_d)
        cload(cs[:], cs_d)
        cload(par[:], bass.AP(par_d.tensor, 0, [[0, 128], [1, NPAR]]))

        def load_x(m):
            for s in range(4):
                r0 = m * T + s * 128
                P.dma(sp, mk("dma_start", out=xs[m % 2][s][:], in_=x_d[r0:r0 + 128, :]),
                      xs[m % 2][s].b, writes=[xs[m % 2][s].b])

        load_x(0)
        for k in range(8):
            P.dma(pool, mk("dma_start", out=WIT[:, k, :], in_=wtm_d[k * 128:(k + 1) * 128, :]),
                  WIT.b, writes=[WIT.b], deps=False)
        for k in range(8):
            P.dma(pool, mk("dma_start",
                out=scfm_d[:, :, k * 128:(k + 1) * 128].rearrange("b p c -> p b c"),
                in_=wfm_d[k * 128:(k + 1) * 128, :].rearrange("p (b c) -> p b c", c=128)),
                b_scfm, writes=[b_scfm], deps=False)
        for k in range(8):
            P.dma(pool, mk("dma_start", out=WO[:, k, :], in_=wo_d[k * 128:(k + 1) * 128, :]),
                  WO.b, writes=[WO.b], deps=False)
        for (src, dst, bb) in ((wg_d, scg_d, b_scg), (wu_d, scu_d, b_scu)):
            for k in range(8):
                P.dma(pool, mk("dma_start",
                    out=dst[:, :, k * 128:(k + 1) * 128].rearrange("b p c -> p b c"),
                    in_=src[k * 128:(k + 1) * 128, :].rearrange("p (b c) -> p b c", c=128)),
                    bb, writes=[bb], deps=False)
        for f in range(NFF):
            P.dma(pool, mk("dma_start", out=scd_d[f * 128:(f + 1) * 128, :],
                                                  in_=wd_d[f * 128:(f + 1) * 128, :]),
                  b_scd, writes=[b_scd], deps=False)

        P.op(dve, mk("tensor_scalar", out=g12[:], in0=g12[:], scalar1=32.0, scalar2=None, op0=ALU.mult),
             reads=[b_c], writes=[b_c])
        P.op(dve, mk("tensor_scalar", out=par[:, 16:656], in0=par[:, 16:656], scalar1=8.0, scalar2=None, op0=ALU.mult),
             reads=[b_c], writes=[b_c])
        P.op(dve, mk("memset", C32[:], 0.0), writes=[C32.b])
        P.op(dve, mk("memset", Mst[:], 1.0), writes=[Mst.b])
        P.op(pool, mk("memset", neghalf[:], -0.5), writes=[neghalf.b])
        P.op(dve, mk("memset", halo[:], 0.0), writes=halo.b)
        for i in range(3):
            P.op(dve, mk("memset", Vaug[i][:], 1.0), writes=[Vaug[i].b])
        P.op(act, mk("activation", out=esink[:], in_=sinks_ap(), func=AF.Exp), reads=[b_c], writes=[esink.b])

        ring_i = [0]
        dring_i = [0]

        fring_i = [0]

        def ring_load(src_ap, srcbuf, fm=False):
            if fm:
                slot = fring[fring_i[0] % 2]
                fring_i[0] += 1
            else:
                slot = ring[ring_i[0] % NR]
                ring_i[0] += 1
            P.dma(sp, mk("dma_start", out=slot[:], in_=src_ap.rearrange("p (k c) -> p k c", c=128)),
                  slot.b, reads=[srcbuf], writes=[slot.b])
            return slot

        def dring_load(f0, half):
            slot = dring[dring_i[0] % ND]
            dring_i[0] += 1
            P.dma(sp, mk("dma_start",
                out=slot[:], in_=scd_d[f0 * 128:(f0 + 2) * 128, half * 512:(half + 1) * 512]
                .rearrange("(f p) c -> p f c", p=128)),
                slot.b, reads=[b_scd], writes=[slot.b])
            return slot

        def rsqrt_pool(dst, src, n, scale):
            P.op(pool, mk("tensor_scalar", out=dst[:, 0:n], in0=src[:, 0:n], scalar1=EPS / scale, scalar2=None,
                          op0=ALU.add), reads=[src.b], writes=[dst.b])
            P.op(pool, mk("tensor_tensor", out=dst[:, 0:n], in0=dst[:, 0:n], in1=neghalf[:, 0:n], op=ALU.pow),
                 reads=[dst.b, neghalf.b], writes=[dst.b])

        def rms_A(xt, par_i):
            u = ubf[par_i]
            P.op(act, mk("activation", out=u[:], in_=xt[:], func=AF.Square, accum_out=ssq[:]),
                 reads=[xt.b], writes=[u.b, ssq.b])
            rsqrt_pool(rstd, ssq, 1, 1.0 / D_MODEL)
            P.op(act, mk("activation", out=u[:], in_=xt[:], func=AF.Copy, scale=rstd[:]),
                 reads=[xt.b, rstd.b], writes=[u.b])

        def rms_B(gcol, dstT, s, par_i):
            u = ubf[par_i]
            bk = nb_()
            bv = bk[:].bitcast(BF)
            for k in range(8):
                P.op(pe, mk("transpose", out=bv[:, k * 128:(k + 1) * 128], in_=u[:, k * 128:(k + 1) * 128],
                                                    identity=ident[:]),
                     reads=[u.b, b_c], writes=[bk.b])
            P.op(dve, mk("tensor_tensor",
                out=dstT[:, :, s * 128:(s + 1) * 128], in0=bv.rearrange("p (k t) -> p k t", k=8),
                in1=g12[:, gcol:gcol + 8].unsqueeze(2).to_broadcast([128, 8, 128]), op=ALU.mult),
                reads=[bk.b, b_c], writes=[dstT.b[s]])

        def transpose_to(src, nblk, dst_ap, dst_bufs, extra=None):
            bk = nb_()
            bv = bk[:].bitcast(BF)
            for j in range(nblk):
                P.op(pe, mk("transpose", out=bv[:, j * 128:(j + 1) * 128], in_=src[:, j * 128:(j + 1) * 128],
                                                    identity=ident[:]),
                     reads=[src.b, b_c], writes=[bk.b])
            return bk, bv

        def mixer(m):
            xsm = xs[m % 2]
            if m > 0:
                load_x(m)
            rms_A(xsm[0], 0)
            yield 'A'
            for s in range(4):
                if s + 1 < 4:
                    rms_A(xsm[s + 1], (s + 1) % 2)
                    yield 'A'
                rms_B(0, uT, s, s % 2)
                yield 'A'
            bGa = nb_()
            for s in range(4):
                for k in range(8):
                    P.op(pe, mk("matmul", out=bGa[:, s * 8:(s + 1) * 8], lhsT=uT[:, k, s * 128:(s + 1) * 128],
                                rhs=WIT[:, k, 1792:1800], start=(k == 0), stop=(k == 7)),
                         reads=[uT.b[s], WIT.b], writes=[bGa.b])
            v48 = lambda t: t[:, 0:32].rearrange("p (s g) -> p s g", s=4)
            v44 = lambda t: t[:, 0:16].rearrange("p (s g) -> p s g", s=4)
            P.op(dve, mk("tensor_tensor", out=v48(gsb4), in0=v48(bGa), in1=gbias().unsqueeze(1).to_broadcast([128, 4, 8]),
                         op=ALU.add), reads=[bGa.b, b_c], writes=[gsb4.b])
            P.op(act, mk("activation", out=v44(l14), in_=v48(gsb4)[:, :, 4:8], func=AF.Exp, scale=-1.0),
                 reads=[gsb4.b], writes=[l14.b])
            P.op(act, mk("activation", out=l14[:], in_=l14[:], func=AF.Ln, bias=1.0), reads=[l14.b], writes=[l14.b])
            yield 'A'
            yield 'A'
            bGb = nb_()
            for s in range(4):
                P.op(pe, mk("matmul", out=bGb[:, s * 8:s * 8 + 4], lhsT=tri[:, 0:128], rhs=l14[:, s * 4:(s + 1) * 4],
                            start=True, stop=True), reads=[l14.b, b_c], writes=[bGb.b])
                P.op(pe, mk("matmul", out=bGb[:, s * 8 + 4:s * 8 + 8], lhsT=tri[:, 128:256], rhs=l14[:, s * 4:(s + 1) * 4],
                            start=True, stop=True), reads=[l14.b, b_c], writes=[bGb.b])
            P.op(dve, mk("tensor_copy", out=gq4[:], in_=bGb[:, 0:32]), reads=[bGb.b], writes=[gq4.b])
            P.op(dve, mk("tensor_tensor", out=v44(apr4), in0=v48(gsb4)[:, :, 0:4], in1=v48(gq4)[:, :, 0:4], op=ALU.add),
                 reads=[gsb4.b, gq4.b], writes=[apr4.b])
            P.op(act, mk("activation", out=E4[:], in_=apr4[:], func=AF.Exp), reads=[apr4.b], writes=[E4.b])
            P.op(act, mk("activation", out=v48(XG)[:, :, 0:4], in_=v48(gq4)[:, :, 0:4], func=AF.Exp),
                 reads=[gq4.b], writes=[XG.b])
            P.op(act, mk("activation", out=v48(XG)[:, :, 4:8], in_=v48(gq4)[:, :, 4:8], func=AF.Exp, scale=-1.0),
                 reads=[gq4.b], writes=[XG.b])
            yield 'A'
            yield 'A'
            bGc = nb_()
            for s in range(4):
                P.op(pe, mk("matmul", out=bGc[:, s * 4:(s + 1) * 4], lhsT=tri[:, 128:256], rhs=E4[:, s * 4:(s + 1) * 4],
                            start=True, stop=True), reads=[E4.b, b_c], writes=[bGc.b])
            P.op(dve, mk("tensor_copy", out=S4[:], in_=bGc[:, 0:16]), reads=[bGc.b], writes=[S4.b])
            for s in range(4):
                c4 = slice(s * 4, (s + 1) * 4)
                P.op(dve, mk("tensor_tensor", out=Mx[:], in0=S4[:, c4], in1=Mst[:], op=ALU.max),
                     reads=[S4.b, Mst.b], writes=[Mx.b])
                P.op(dve, mk("reciprocal", out=inv4[:], in_=Mx[:]), reads=[Mx.b], writes=[inv4.b])
                P.op(dve, mk("tensor_tensor", out=W3[:, s, 0:4], in0=E4[:, c4], in1=inv4[:], op=ALU.mult),
                     reads=[E4.b, inv4.b], writes=[W3.b[s]])
                P.op(dve, mk("tensor_tensor", out=W3[:, s, 4:8], in0=Mst[:], in1=inv4[:], op=ALU.mult),
                     reads=[Mst.b, inv4.b], writes=[W3.b[s]])
                P.op(dve, mk("scalar_tensor_tensor", out=W3[:, s, 8:12], in0=XG[:, s * 8:s * 8 + 4], scalar=2.0, in1=inv4[:],
                             op0=ALU.mult, op1=ALU.mult), reads=[XG.b, inv4.b], writes=[W3.b[s]])
                P.op(dve, mk("tensor_tensor", out=Mst[:], in0=Mx[:], in1=XG[:, s * 8 + 4:s * 8 + 8], op=ALU.mult),
                     reads=[Mx.b, XG.b], writes=[Mst.b])
            yield 'A'
            slots = {}
            for blk in range(2):
                slots[blk] = ring_load(scfm_d[blk], b_scfm, fm=True)
            for blk in range(8):
                slot = slots[blk]
                bk = nb_()
                for k in range(8):
                    P.op(pe, mk("matmul", out=bk[:], lhsT=slot[:, k, :], rhs=uT[:, k, :],
                                                                      start=(k == 0), stop=(k == 7)),
                         reads=[slot.b] + uT.b, writes=[bk.b])
                pr = pre[blk % 2]
                y = yc[blk % 2]
                P.op(act, mk("activation", out=pr[:, 3:T + 3], in_=bk[:], func=AF.Copy),
                     reads=[bk.b], writes=[pr.b])
                P.op(dve, mk("tensor_copy", out=pr[:, 0:3], in_=halo[:, blk, :]),
                     reads=[halo.b[blk]], writes=[pr.b])
                P.op(dve, mk("tensor_scalar",
                    out=y[:], in0=pr[:, 0:T], scalar1=cw[:, blk * 4:blk * 4 + 1], scalar2=None, op0=ALU.mult),
                    reads=[pr.b, b_c], writes=[y.b])
                for j in range(1, 4):
                    P.op(dve, mk("scalar_tensor_tensor",
                        out=y[:], in0=pr[:, j:T + j], scalar=cw[:, blk * 4 + j:blk * 4 + j + 1], in1=y[:],
                        op0=ALU.mult, op1=ALU.add),
                        reads=[pr.b, b_c, y.b], writes=[y.b])
                P.op(dve, mk("tensor_copy", out=halo[:, blk, :], in_=pr[:, T:T + 3]),
                     reads=[pr.b], writes=[halo.b[blk]])
                P.op(act, mk("activation", out=pr[:, 0:T], in_=y[:], func=AF.Tanh, scale=0.5),
                     reads=[y.b], writes=[pr.b])
                P.op(dve, mk("scalar_tensor_tensor", out=qkT[:, blk, :], in0=pr[:, 0:T], scalar=1.0, in1=y[:],
                             op0=ALU.add, op1=ALU.mult),
                     reads=[pr.b, y.b], writes=[qkT.b[blk]])
                if blk + 2 < 8:
                    slots[blk + 2] = ring_load(scfm_d[blk + 2], b_scfm, fm=True)
                yield 'A'

            def tm_proj(s, cols):
                sl = slice(s * 128, (s + 1) * 128)
                tb = []
                for (c0, c1) in cols:
                    bk = nb_()
                    tb.append(bk)
                    for k in range(8):
                        P.op(pe, mk("matmul",
                            out=bk[:, 0:c1 - c0], lhsT=uT[:, k, sl], rhs=WIT[:, k, c0:c1],
                            start=(k == 0), stop=(k == 7)),
                            reads=[uT.b[s], WIT.b], writes=[bk.b])
                return tb

            def mlstm_chain(s):
                gt = m * 4 + s
                sl = slice(s * 128, (s + 1) * 128)
                Bv, Bo = tm_proj(s, [(0, 512), (512, 1024)])
                P.op(act, mk("activation", out=v_sb[:], in_=Bv[:], func=AF.Copy), reads=[Bv.b], writes=[v_sb.b])
                P.op(act, mk("activation", out=og[:], in_=Bo[:], func=AF.Exp, scale=-1.0), reads=[Bo.b], writes=[og.b])
                P.op(dve, mk("tensor_scalar", out=og[:], in0=og[:], scalar1=1.0, scalar2=None, op0=ALU.add),
                     reads=[og.b], writes=[og.b])
                P.op(dve, mk("reciprocal", out=og[:], in_=og[:]), reads=[og.b], writes=[og.b])
                P.op(dve, mk("tensor_tensor", out=og[:], in0=og[:], in1=gm_ap(), op=ALU.mult),
                     reads=[og.b, b_c], writes=[og.b])

                yield
                kscale = 0.5 * 128.0 ** -0.5
                P.op(dve, mk("scalar_tensor_tensor",
                    out=vt[:, :, 0:128], in0=v_sb[:].rearrange("p (h d) -> p h d", h=4), scalar=kscale,
                    in1=W3[:, s, 0:4].unsqueeze(2).to_broadcast([128, 4, 128]), op0=ALU.mult, op1=ALU.mult),
                    reads=[v_sb.b, W3.b[s]], writes=[vt.b])
                P.op(dve, mk("tensor_scalar", out=vt[:, :, 128:129], in0=W3[:, s, 0:4].unsqueeze(2), scalar1=kscale,
                                                    scalar2=None, op0=ALU.mult),
                     reads=[W3.b[s]], writes=[vt.b])
                P.op(dve, mk("tensor_tensor", out=C32[:], in0=C32[:],
                                                    in1=W3[:, s, 4:8].unsqueeze(2).to_broadcast([128, 4, 129]), op=ALU.mult),
                     reads=[C32.b, W3.b[s]], writes=[C32.b])
                P.op(act, mk("activation", out=Cbf[:], in_=C32[:], func=AF.Copy), reads=[C32.b], writes=[Cbf.b])

                yield
                bKt = nb_()
                bKv = bKt[:].bitcast(BF)
                for h in range(4):
                    P.op(pe, mk("transpose", out=bKv[:, h * 128:(h + 1) * 128], in_=qkT[:, 4 + h, sl],
                                                        identity=ident[:]),
                         reads=[qkT.b[4 + h], b_c], writes=[bKt.b])
                P.op(act, mk("activation", out=k_tm[:], in_=bKv[:, 0:512], func=AF.Copy), reads=[bKt.b], writes=[k_tm.b])
                bS = nb_()
                for h in range(4):
                    P.op(pe, mk("matmul", out=bS[:, h * 128:(h + 1) * 128], lhsT=qkT[:, 4 + h, sl],
                                                     rhs=qkT[:, h, sl], start=True, stop=True),
                         reads=[qkT.b[4 + h], qkT.b[h]], writes=[bS.b])
                P.op(dve, mk("tensor_tensor",
                    out=Sm[:], in0=bS[:].rearrange("p (h l) -> p h l", h=4),
                    in1=masks[:, 0:128].unsqueeze(1).to_broadcast([128, 4, 128]), op=ALU.mult),
                    reads=[bS.b, b_c], writes=[Sm.b])
                yield
                yield
                yield
                bN = [nb_(), nb_()]
                for h in range(4):
                    bk = bN[h // 2]
                    o0 = (h % 2) * 129
                    P.op(pe, mk("matmul", out=bk[:, o0:o0 + 129], lhsT=Sm[:, h, :], rhs=vt[:, h, :],
                                                                   start=True, stop=False),
                         reads=[Sm.b, vt.b], writes=[bk.b])
                    P.op(pe, mk("matmul", out=bk[:, o0:o0 + 129], lhsT=qkT[:, h, sl], rhs=Cbf[:, h, :],
                                                                   start=False, stop=True),
                         reads=[qkT.b[h], Cbf.b], writes=[bk.b])
                bD = [nb_(), nb_()]
                for h in range(4):
                    bk = bD[h // 2]
                    o0 = (h % 2) * 129
                    P.op(pe, mk("matmul", out=bk[:, o0:o0 + 129], lhsT=k_tm[:, h * 128:(h + 1) * 128],
                                                                   rhs=vt[:, h, :], start=True, stop=True),
                         reads=[k_tm.b, vt.b], writes=[bk.b])
                for i in range(2):
                    P.op(dve, mk("tensor_tensor",
                        out=C32[:, 2 * i:2 * i + 2, :], in0=C32[:, 2 * i:2 * i + 2, :],
                        in1=bD[i][:, 0:258].rearrange("p (h e) -> p h e", h=2), op=ALU.add),
                        reads=[C32.b, bD[i].b], writes=[C32.b])
                den4 = dd
                for i in range(2):
                    P.op(act, mk("activation", out=hmf[:, 256 * i:256 * i + 256].rearrange("p (h e) -> p h e", h=2),
                                 in_=bN[i][:, 0:258].rearrange("p (h e) -> p h e", h=2)[:, :, 0:128], func=AF.Copy),
                         reads=[bN[i].b], writes=[hmf.b])
                    P.op(dve, mk("tensor_copy", out=t4[:, 2 * i:2 * i + 2].unsqueeze(2),
                                 in_=bN[i][:, 0:258].rearrange("p (h e) -> p h e", h=2)[:, :, 128:129]),
                         reads=[bN[i].b], writes=[t4.b])
                yield
                P.op(dve, mk("tensor_tensor", out=dd[:], in0=t4[:], in1=W3[:, s, 8:12], op=ALU.max),
                     reads=[t4.b, W3.b[s]], writes=[dd.b])
                P.op(dve, mk("scalar_tensor_tensor", out=dd[:], in0=t4[:], scalar=-1.0, in1=dd[:], op0=ALU.mult, op1=ALU.max),
                     reads=[t4.b, dd.b], writes=[dd.b])
                for h in range(4):
                    P.op(act, mk("activation", out=hm[:, 0:128], in_=hmf[:, h * 128:(h + 1) * 128],
                                 func=AF.Square, accum_out=ss4[:, h:h + 1]),
                         reads=[hmf.b], writes=[hm.b, ss4.b])
                P.op(dve, mk("scalar_tensor_tensor", out=rd[:], in0=dd[:], scalar=EPS, in1=dd[:], op0=ALU.mult, op1=ALU.mult),
                     reads=[dd.b], writes=[rd.b])
                P.op(dve, mk("scalar_tensor_tensor", out=sc4[:], in0=ss4[:], scalar=1.0 / 128, in1=rd[:], op0=ALU.mult, op1=ALU.add),
                     reads=[ss4.b, rd.b], writes=[sc4.b])
                P.op(pool, mk("tensor_tensor", out=sc4[:], in0=sc4[:], in1=neghalf[:, 0:4], op=ALU.pow),
                     reads=[sc4.b, neghalf.b], writes=[sc4.b])
                yield
                for h in range(4):
                    hs = slice(h * 128, (h + 1) * 128)
                    P.op(dve, mk("scalar_tensor_tensor", out=hm[:, hs], in0=hmf[:, hs], scalar=sc4[:, h:h + 1], in1=og[:, hs],
                                 op0=ALU.mult, op1=ALU.mult),
                         reads=[hmf.b, sc4.b, og.b], writes=[hm.b])
                yield
                yield
                bk, bv = transpose_to(hm, 4, None, None)
                P.op(act, mk("activation", out=hcatT[:, 0:4, sl], in_=bv[:, 0:512].rearrange("p (k t) -> p k t", k=4), func=AF.Copy),
                     reads=[bk.b], writes=[hcatT.b[s]])
                yield

            def swa_front(s):
                gt = m * 4 + s
                QT = QT2[gt % 2]
                Bq, Bk = tm_proj(s, [(1024, 1536), (1536, 1792)])
                P.op(act, mk("activation", out=qk_sb[:, 0:512], in_=Bq[:], func=AF.Copy), reads=[Bq.b], writes=[qk_sb.b])
                P.op(act, mk("activation", out=qk_sb[:, 512:640], in_=Bk[:, 0:128], func=AF.Copy),
                     reads=[Bk.b], writes=[qk_sb.b])
                Va = Vaug[gt % 3]
                P.op(act, mk("activation", out=Va[:, :, 0:64],
                                                      in_=Bk[:, 128:256].rearrange("p (g d) -> p g d", g=2), func=AF.Copy),
                     reads=[Bk.b], writes=[Va.b])
                yield
                P.op(dve, mk("tensor_tensor", out=sq[:], in0=qk_sb[:], in1=qk_sb[:], op=ALU.mult),
                     reads=[qk_sb.b], writes=[sq.b])
                P.op(dve, mk("tensor_reduce", out=ssq10[:], in_=sq[:].rearrange("p (h d) -> p h d", h=10),
                                                    axis=AX.X, op=ALU.add),
                     reads=[sq.b], writes=[ssq10.b])
                rsqrt_pool(r10, ssq10, 10, 1.0 / 64)
                P.op(dve, mk("tensor_tensor",
                    out=sq[:].rearrange("p (h d) -> p h d", h=10), in0=qk_sb[:].rearrange("p (h d) -> p h d", h=10),
                    in1=r10[:].unsqueeze(2).to_broadcast([128, 10, 64]), op=ALU.mult),
                    reads=[qk_sb.b, r10.b], writes=[sq.b])
                P.op(dve, mk("tensor_tensor", out=sq[:], in0=sq[:], in1=gqk_ap(), op=ALU.mult),
                     reads=[sq.b, b_c], writes=[sq.b])
                P.op(dve, mk("tensor_copy", out=qkn[:], in_=sq[:]), reads=[sq.b], writes=[qkn.b])
                sq3 = lambda: sq[:].rearrange("p (h d) -> p h d", h=10)
                qn3 = lambda: qkn[:].rearrange("p (h d) -> p h d", h=10)
                cosb = lambda: cs[:, gt * 16:gt * 16 + 8].unsqueeze(1).to_broadcast([128, 10, 8])
                sinb = lambda: cs[:, gt * 16 + 8:gt * 16 + 16].unsqueeze(1).to_broadcast([128, 10, 8])
                rt3 = lambda i: rt[:, i, :].rearrange("p (h d) -> p h d", h=10)
                P.op(dve, mk("tensor_tensor", out=rt3(0), in0=sq3()[:, :, 0:8], in1=cosb(), op=ALU.mult),
                     reads=[sq.b, b_c], writes=[rt.b])
                P.op(dve, mk("tensor_tensor", out=rt3(1), in0=sq3()[:, :, 8:16], in1=sinb(), op=ALU.mult),
                     reads=[sq.b, b_c], writes=[rt.b])
                P.op(dve, mk("tensor_tensor", out=rt3(2), in0=sq3()[:, :, 8:16], in1=cosb(), op=ALU.mult),
                     reads=[sq.b, b_c], writes=[rt.b])
                P.op(dve, mk("tensor_tensor", out=rt3(3), in0=sq3()[:, :, 0:8], in1=sinb(), op=ALU.mult),
                     reads=[sq.b, b_c], writes=[rt.b])
                P.op(dve, mk("tensor_tensor", out=qn3()[:, :, 0:8], in0=rt3(0), in1=rt3(1), op=ALU.subtract),
                     reads=[rt.b], writes=[qkn.b])
                P.op(dve, mk("tensor_tensor", out=qn3()[:, :, 8:16], in0=rt3(2), in1=rt3(3), op=ALU.add),
                     reads=[rt.b], writes=[qkn.b])
                yield
                yield
                bk, bv = transpose_to(qkn, 5, None, None)
                KTc = KT[gt % 3]
                P.op(act, mk("activation", out=QT[:], in_=bv[:, 0:512].rearrange("p (k t) -> p k t", k=4), func=AF.Copy),
                     reads=[bk.b], writes=[QT.b])
                P.op(dve, mk("tensor_copy", out=KTc[:], in_=bv[:, 512:640]), reads=[bk.b], writes=[KTc.b])
                yield

            def swa_back(s):
                gt = m * 4 + s
                sl = slice(s * 128, (s + 1) * 128)
                QT = QT2[gt % 2]
                KTc, Va = KT[gt % 3], Vaug[gt % 3]
                blocks = [(KTc, Va, 0)] if gt == 0 else [(KT[(gt - 1) % 3], Vaug[(gt - 1) % 3], 128), (KTc, Va, 0)]
                for g in range(2):
                    ps_ = slice(g * 64, (g + 1) * 64)
                    for bi, (Kt, Vt, moff) in enumerate(blocks):
                        idx = g * 2 + bi
                        bSc = nb_()
                        P.op(pe, mk("matmul", out=bSc[:], lhsT=Kt[ps_, :], rhs=QT[ps_, :, :],
                                                                            start=True, stop=False),
                             reads=[Kt.b, QT.b], writes=[bSc.b])
                        P.op(pe, mk("matmul", out=bSc[:], lhsT=ident[:],
                                    rhs=negm[:, moff:moff + 128].unsqueeze(1).to_broadcast([128, 4, 128]),
                                    start=False, stop=True),
                             reads=[b_c], writes=[bSc.b])
                        P.op(act, mk("activation", out=PT[:, idx, :], in_=bSc[:], func=AF.Exp, scale=0.125),
                             reads=[bSc.b], writes=[PT.b[idx]])
                yield
                bO = [nb_(), nb_()]
                for h in range(8):
                    g, j = h // 4, h % 4
                    for bi, (Kt, Vt, moff) in enumerate(blocks):
                        idx = g * 2 + bi
                        P.op(pe, mk("matmul",
                            out=bO[g][:, j * 65:(j + 1) * 65], lhsT=PT[:, idx, j * 128:(j + 1) * 128], rhs=Vt[:, g, :],
                            start=(bi == 0), stop=(bi == len(blocks) - 1)),
                            reads=[PT.b[idx], Vt.b], writes=[bO[g].b])
                for g in range(2):
                    P.op(dve, mk("tensor_tensor",
                        out=den8[:, 4 * g:4 * g + 4].unsqueeze(2),
                        in0=bO[g][:, 0:260].rearrange("p (h e) -> p h e", h=4)[:, :, 64:65],
                        in1=esink[:, 4 * g:4 * g + 4].unsqueeze(2), op=ALU.add),
                        reads=[bO[g].b, esink.b], writes=[den8.b])
                P.op(dve, mk("reciprocal", out=rden8[:], in_=den8[:]), reads=[den8.b], writes=[rden8.b])
                for g in range(2):
                    P.op(dve, mk("tensor_tensor",
                        out=ha[:, 256 * g:256 * g + 256].rearrange("p (h d) -> p h d", h=4),
                        in0=bO[g][:, 0:260].rearrange("p (h e) -> p h e", h=4)[:, :, 0:64],
                        in1=rden8[:, 4 * g:4 * g + 4].unsqueeze(2).to_broadcast([128, 4, 64]), op=ALU.mult),
                        reads=[bO[g].b, rden8.b], writes=[ha.b])
                yield
                yield
                bk, bv = transpose_to(ha, 4, None, None)
                P.op(act, mk("activation", out=hcatT[:, 4:8, sl], in_=bv[:, 0:512].rearrange("p (k t) -> p k t", k=4),
                                                      func=AF.Copy),
                     reads=[bk.b], writes=[hcatT.b[s]])


            def join_chain(s):
                sl = slice(s * 128, (s + 1) * 128)
                xt = xsm[s]
                for half in range(2):
                    bk = nb_()
                    for k in range(8):
                        P.op(pe, mk("matmul",
                            out=bk[:], lhsT=hcatT[:, k, sl], rhs=WO[:, k, half * 512:(half + 1) * 512],
                            start=(k == 0), stop=(k == 7)),
                            reads=[hcatT.b[s], WO.b], writes=[bk.b])
                    P.op(dve, mk("tensor_tensor",
                        out=xt[:, half * 512:(half + 1) * 512], in0=bk[:], in1=xt[:, half * 512:(half + 1) * 512], op=ALU.add),
                        reads=[bk.b, xt.b], writes=[xt.b])
                yield
                rms_A(xt, s % 2)
                yield
                yield
                yield
                yield
                rms_B(8, u2T, s, s % 2)
                yield

            def rr(gens):
                gens = list(gens)
                while gens:
                    for g in list(gens):
                        try:
                            next(g)
                            yield 'B'
                        except StopIteration:
                            gens.remove(g)
            yield from rr([swa_front(0)])
            for s in range(4):
                yield from rr([mlstm_chain(s), swa_back(s)] + ([join_chain(s - 1)] if s > 0 else [])
                              + ([swa_front(s + 1)] if s + 1 < 4 else []))
            yield from rr([join_chain(3)])

        def ffn(m):
            xsm = xs[m % 2]
            ffn_slots = {}

            def ffn_fetch(f):
                if f < NFF and f not in ffn_slots:
                    ffn_slots[f] = (ring_load(scg_d[f], b_scg), ring_load(scu_d[f], b_scu))
            for f in range(NR // 2):
                ffn_fetch(f)
            dslots = {}

            def d_fetch(i):
                if i < 22 and i not in dslots:
                    dslots[i] = dring_load((i % 11) * 2, i // 11)
            for f in range(NFF):
                ffn_fetch(f)
                sg_, su_ = ffn_slots[f]
                bGt = nbf_()
                bUp = nbf_()
                for (slot, bk) in ((sg_, bGt), (su_, bUp)):
                    for k in range(8):
                        P.op(pe, mk("matmul", out=bk[:], lhsT=slot[:, k, :], rhs=u2T[:, k, :],
                                                                          start=(k == 0), stop=(k == 7)),
                             reads=[slot.b] + u2T.b, writes=[bk.b])
                    if bk is bGt:
                        yield 1.7
                ffn_fetch(f + NR // 2)
                sgt = sg[f % 2]
                P.op(act, mk("activation", out=sgt[:], in_=bGt[:], func=AF.Tanh, scale=0.5),
                     reads=[bGt.b], writes=[sgt.b])
                P.op(dve, mk("scalar_tensor_tensor", out=sgt[:], in0=sgt[:], scalar=1.0, in1=bGt[:],
                             op0=ALU.add, op1=ALU.mult),
                     reads=[sgt.b, bGt.b], writes=[sgt.b])
                P.op(dve, mk("scalar_tensor_tensor", out=actT[:, f, :], in0=sgt[:], scalar=0.5, in1=bUp[:],
                             op0=ALU.mult, op1=ALU.mult),
                     reads=[sgt.b, bUp.b], writes=[actT.b[f]])
                if f >= NFF - ND:
                    d_fetch(f - (NFF - ND))
                yield 3.5
            for half in range(2):
                bY = [nbf_() for _ in range(4)]
                for f in range(NFF):
                    i = half * 11 + f // 2
                    d_fetch(i)
                    slot = dslots[i]
                    for s in range(4):
                        P.op(pe, mk("matmul",
                            out=bY[s][:], lhsT=actT[:, f, s * 128:(s + 1) * 128], rhs=slot[:, f % 2, :],
                            start=(f == 0), stop=(f == NFF - 1)),
                            reads=[actT.b[f], slot.b], writes=[bY[s].b])
                    if f % 2 == 1:
                        d_fetch(i + ND)
                    yield 0.85
                for s in range(4):
                    P.op(dve, mk("tensor_tensor",
                        out=xsm[s][:, half * 512:(half + 1) * 512], in0=bY[s][:], in1=xsm[s][:, half * 512:(half + 1) * 512],
                        op=ALU.add),
                        reads=[bY[s].b, xsm[s].b], writes=[xsm[s].b])
            for s in range(4):
                r0 = m * T + s * 128
                P.dma(sp, mk("dma_start", out=out_d[r0:r0 + 128, :], in_=xsm[s][:]),
                      xsm[s].b, reads=[xsm[s].b], writes=[])
            yield 0.01

        def drive(*gens):
            gens = list(gens)
            while gens:
                for g in list(gens):
                    try:
                        next(g)
                    except StopIteration:
                        gens.remove(g)

        drive(mixer(0))
        for m in range(NM):
            if m + 1 < NM:
                drive(ffn(m), mixer(m + 1))
            else:
                drive(ffn(m))
        P.wait_all(sp, [x.b for xx in xs for x in xx])
        P.emit()
    return nc


def _host_layout(inputs, NT):
    f32 = np.float32
    w_in = np.asarray(inputs["w_in"], f32)[0]
    qperm = np.concatenate([np.arange(h * 64, (h + 1) * 64) for h in (0, 4, 1, 5, 2, 6, 3, 7)])
    cols_tm = np.concatenate([
        np.arange(1024, 1536), np.arange(1536, 2048), 2056 + qperm, np.arange(2568, 2696),
        np.arange(2696, 2824), np.arange(2048, 2056)])
    w_in_tm = np.ascontiguousarray(w_in[:, cols_tm])
    w_in_fm = np.ascontiguousarray(w_in[:, 0:1024])
    params = np.concatenate([
        np.asarray(inputs["igate_b"], f32)[0], np.asarray(inputs["fgate_b"], f32)[0],
        np.asarray(inputs["sinks"], f32)[0],
        np.tile(np.asarray(inputs["q_norm_g"], f32)[0], 8), np.tile(np.asarray(inputs["k_norm_g"], f32)[0], 2),
        np.asarray(inputs["mlstm_norm_g"], f32)[0]]).reshape(1, NPAR)
    g12 = np.ascontiguousarray(np.concatenate([
        np.asarray(inputs["norm1_g"], f32)[0].reshape(8, 128).T,
        np.asarray(inputs["norm2_g"], f32)[0].reshape(8, 128).T], axis=1))
    convw = np.ascontiguousarray(np.asarray(inputs["conv_w"], f32)[0].reshape(4, 8, 128).transpose(2, 1, 0).reshape(128, 32))
    pos = np.arange(NT, dtype=f32)
    inv_freq = (f32(500000.0) ** (-np.arange(0, 16, 2, dtype=f32) / f32(16))).astype(f32)
    ang = (pos[:, None] * inv_freq[None, :]).astype(f32)
    cs_ = np.concatenate([np.cos(ang), np.sin(ang)], axis=1).astype(f32)
    ropecs = np.ascontiguousarray(cs_.reshape(NT // 128, 128, 16).transpose(1, 0, 2).reshape(128, -1))
    tri_le = np.triu(np.ones((128, 128), f32))
    masks = np.concatenate([tri_le, 1.0 - tri_le], axis=1).astype(ml_dtypes.bfloat16)
    negmask = ((np.concatenate([tri_le, 1.0 - tri_le], axis=1) - 1.0) * 30000.0).astype(ml_dtypes.bfloat16)
    tri = np.concatenate([tri_le, np.ones((128, 128), f32)], axis=1)
    ident = np.eye(128, dtype=f32).astype(ml_dtypes.bfloat16)
    return dict(
        w_in_tm=w_in_tm, w_in_fm=w_in_fm, w_out=np.asarray(inputs["w_out"], f32)[0],
        w_gate=np.asarray(inputs["w_gate"], f32)[0], w_up=np.asarray(inputs["w_up"], f32)[0],
        w_down=np.asarray(inputs["w_down"], f32)[0], params=params, g12=g12, convw=convw, ropecs=ropecs,
        ident=ident, masks=masks, negmask=negmask, tri=tri)


def run(inputs, n_cores=8, NT=4096):
    x = np.asarray(inputs["x"], np.float32)
    shared = _host_layout(inputs, NT)
    nc = build_nc(NT)
    in_maps = [dict(shared, x=np.ascontiguousarray(x[b, :NT])) for b in range(n_cores)]
    res = run_bass_kernel_spmd(nc, in_maps, core_ids=list(range(n_cores)))
    return np.stack([np.asarray(r["out"], np.float32) for r in res.results], axis=0)


def kernel(**inputs):
    return run(inputs, n_cores=8, NT=4096)
```

```python
import math
import sys
from contextlib import ExitStack

import numpy as np
import ml_dtypes

import concourse.bass as bass
import concourse.mybir as mybir
from concourse.bass_utils import run_bass_kernel_spmd

F32 = mybir.dt.float32
BF = mybir.dt.bfloat16
ALU = mybir.AluOpType
AF = mybir.ActivationFunctionType
AX = mybir.AxisListType

D_MODEL = 1024
D_FF = 2816
NFF = D_FF // 128
EPS = 1e-6
N_TM = 1800
NPAR = 1168


class Buf:
    __slots__ = ("name", "w", "r", "excl", "dsem", "dcount")

    def __init__(self, name, excl=False):
        self.name = name
        self.w = None
        self.r = {}
        self.excl = excl
        self.dsem = None
        self.dcount = 0


class Eng:
    def __init__(self, name, eng, sem, nowait_self=False):
        self.name = name
        self.eng = eng
        self.sem = sem
        self.n = 0
        self.ops = []
        self.waited = {}
        self.nowait_self = nowait_self


class Prog:
    def __init__(self, nc, stack):
        self.nc = nc
        self.stack = stack
        mk = lambda n: stack.enter_context(nc.semaphore(n))
        self.pe = Eng("pe", nc.tensor, mk("s_pe"), nowait_self=True)
        self.act = Eng("act", nc.scalar, mk("s_act"))
        self.dve = Eng("dve", nc.vector, mk("s_dve"))
        self.pool = Eng("pool", nc.gpsimd, mk("s_pool"))
        self.sp = Eng("sp", nc.sync, mk("s_sp"))

    def new_sem(self, name):
        return self.stack.enter_context(self.nc.semaphore(name))

    def _deps(self, E, reads, writes):
        toks = {}

        def add(t):
            if t is None:
                return
            s, v = t
            k = id(s)
            if k not in toks or toks[k][1] < v:
                toks[k] = (s, v)
        for b in reads:
            add(b.w)
        for b in writes:
            add(b.w)
            for t in b.r.values():
                add(t)
        out = []
        for k, (s, v) in toks.items():
            if E.nowait_self and s is E.sem:
                continue
            if E.waited.get(k, 0) >= v:
                continue
            E.waited[k] = v
            out.append((s, v))
        return out

    def _commit(self, tok, reads, writes):
        for b in writes:
            b.w = tok
            b.r = {}
        for b in reads:
            if b not in writes:
                k = id(tok[0])
                b.r[k] = tok

    def op(self, E, fn, reads=(), writes=()):
        reads = list(reads)
        writes = list(writes)
        for b in list(reads):
            if b.excl and b not in writes:
                writes.append(b)
        waits = self._deps(E, reads, writes)
        E.n += 1
        tok = (E.sem, E.n)
        E.ops.append((waits, fn, E.sem, 1, sys._getframe(1).f_lineno))
        self._commit(tok, reads, writes)
        return tok

    def dma(self, Q, fn, target, reads=(), writes=(), deps=True):
        waits = self._deps(Q, reads, writes) if deps else []
        if target.dsem is None:
            target.dsem = {}
        if Q.name not in target.dsem:
            target.dsem[Q.name] = [self.new_sem("d_%s_%s" % (target.name, Q.name)), 0]
        ent = target.dsem[Q.name]
        ent[1] += 16
        tok = (ent[0], ent[1])
        Q.ops.append((waits, fn, ent[0], 16, sys._getframe(1).f_lineno))
        self._commit(tok, reads, writes)
        return tok

    def wait_all(self, E, bufs):
        waits = self._deps(E, [], bufs)
        E.ops.append((waits, None, None, 0, 0))

    def emit(self):
        with self.nc.Block() as block:
            def run(E):
                def body(eng):
                    for waits, fn, sem, inc, ln in E.ops:
                        for (s, v) in waits:
                            eng.wait_ge(s, v)
                        if fn is not None:
                            fn(eng).then_inc(sem, inc).annotate("L%d" % ln)
                return body
            block.tensor(run(self.pe))
            block.scalar(run(self.act))
            block.vector(run(self.dve))
            block.gpsimd(run(self.pool))
            block.sync(run(self.sp))


class Tl:
    def __init__(self, t, b):
        self.t = t
        self.b = b

    def __getitem__(self, k):
        return self.t[k]


def mk(name, *a, **kw):
    return lambda e: getattr(e, name)(*a, **kw)


def build_nc(NT):
    T = 512
    NM = NT // T
    NTILE = NT // 128
    nc = bass.Bass("TRN2", target_bir_lowering=False)
    dt_in = lambda n, s, d=F32: nc.dram_tensor(n, s, d, kind="ExternalInput").ap()
    x_d = dt_in("x", [NT, D_MODEL])
    wtm_d = dt_in("w_in_tm", [D_MODEL, N_TM])
    wfm_d = dt_in("w_in_fm", [D_MODEL, 1024])
    wo_d = dt_in("w_out", [D_MODEL, D_MODEL])
    wg_d = dt_in("w_gate", [D_MODEL, D_FF])
    wu_d = dt_in("w_up", [D_MODEL, D_FF])
    wd_d = dt_in("w_down", [D_FF, D_MODEL])
    par_d = dt_in("params", [1, NPAR])
    g12_d = dt_in("g12", [128, 16])
    cw_d = dt_in("convw", [128, 32])
    cs_d = dt_in("ropecs", [128, NTILE * 16])
    id_d = dt_in("ident", [128, 128], BF)
    mk_d = dt_in("masks", [128, 256], BF)
    nm_d = dt_in("negmask", [128, 256], BF)
    tri_d = dt_in("tri", [128, 256])
    out_d = nc.dram_tensor("out", [NT, D_MODEL], F32, kind="ExternalOutput").ap()
    scfm_d = nc.dram_tensor("sc_fm", [8, 128, 1024], BF).ap()
    scg_d = nc.dram_tensor("sc_g", [NFF, 128, 1024], BF).ap()
    scu_d = nc.dram_tensor("sc_u", [NFF, 128, 1024], BF).ap()
    scd_d = nc.dram_tensor("sc_d", [D_FF, D_MODEL], BF).ap()

    with ExitStack() as st:
        P = Prog(nc, st)
        pe, act, dve, pool, sp = P.pe, P.act, P.dve, P.pool, P.sp

        def sb(name, shape, dt, nb=1):
            t = st.enter_context(nc.sbuf_tensor("sb_" + name, shape, dt))
            return Tl(t, Buf(name) if nb == 1 else [Buf(f"{name}{i}") for i in range(nb)])

        banks = [Tl(st.enter_context(nc.psum_tensor(f"bank{i}", [128, 512], F32)), Buf(f"bank{i}", excl=True))
                 for i in range(8)]
        bank_ctr = [0]

        def nb_():
            b = banks[bank_ctr[0] % 4]
            bank_ctr[0] += 1
            return b
        fbank_ctr = [0]

        def nbf_():
            b = banks[4 + fbank_ctr[0] % 4]
            fbank_ctr[0] += 1
            return b

        WIT = sb("WIT", [128, 8, N_TM], BF)
        WO = sb("WO", [128, 8, D_MODEL], BF)
        xs = [[sb(f"x{i}_{s}", [128, D_MODEL], F32) for s in range(4)] for i in range(2)]
        ubf = [sb(f"ubf{i}", [128, D_MODEL], BF) for i in range(2)]
        uT = sb("uT", [128, 8, T], BF, nb=4)
        u2T = sb("u2T", [128, 8, T], BF, nb=4)
        pre = [sb(f"pre{i}", [128, T + 3], F32) for i in range(2)]
        yc = [sb(f"yc{i}", [128, T], F32) for i in range(2)]
        halo = sb("halo", [128, 8, 3], F32, nb=8)
        qkT = sb("qkT", [128, 8, T], BF, nb=8)
        hcatT = sb("hcatT", [128, 8, 256], BF, nb=2)
        actT = sb("actT", [128, NFF, T], BF, nb=NFF)
        sg = [sb(f"sg{i}", [128, T], BF) for i in range(2)]
        NR = 6
        ring = [sb(f"ring{i}", [128, 8, 128], BF) for i in range(NR)]
        ND = 6
        dring = [sb(f"dring{i}", [128, 2, 512], BF) for i in range(ND)]
        fring = [sb(f"fring{i}", [128, 8, 128], BF) for i in range(2)]
        ident = sb("ident", [128, 128], BF)
        masks = sb("masks", [128, 256], BF)
        negm = sb("negm", [128, 256], BF)
        tri = sb("tri", [128, 256], F32)
        par = sb("par", [128, NPAR], F32)
        g12 = sb("g12", [128, 16], F32)
        cw = sb("cw", [128, 32], F32)
        cs = sb("cs", [128, NTILE * 16], F32)
        esink = sb("esink", [128, 8], F32)
        ssq = sb("ssq", [128, 1], F32)
        rstd = sb("rstd", [128, 1], F32)
        v_sb = sb("v_sb", [128, 512], BF)
        vt = sb("vt", [128, 4, 129], BF)
        og = sb("og", [128, 512], F32)
        qk_sb = sb("qk_sb", [128, 640], F32)
        sq = sb("sq", [128, 640], F32)
        qkn = sb("qkn", [128, 640], BF)
        ssq10 = sb("ssq10", [128, 10], F32)
        r10 = sb("r10", [128, 10], F32)
        rt = sb("rt", [128, 4, 80], F32)
        QT2 = [sb(f"QT{i}", [128, 4, 128], BF) for i in range(2)]
        KT = [sb(f"KT{i}", [128, 128], BF) for i in range(3)]
        Vaug = [sb(f"Vaug{i}", [128, 2, 65], BF) for i in range(3)]
        PT = sb("PT", [128, 4, 512], BF, nb=4)
        Sm = sb("Sm", [128, 4, 128], BF)
        k_tm = sb("k_tm", [128, 512], BF)
        hmf = sb("hmf", [128, 512], F32)
        hm = sb("hm", [128, 512], BF)
        ha = sb("ha", [128, 512], BF)
        C32 = sb("C32", [128, 4, 129], F32)
        Cbf = sb("Cbf", [128, 4, 129], BF)
        gsb4 = sb("gsb4", [128, 32], F32)
        l14 = sb("l14", [128, 16], F32)
        gq4 = sb("gq4", [128, 32], F32)
        apr4 = sb("apr4", [128, 16], F32)
        E4 = sb("E4", [128, 16], F32)
        XG = sb("XG", [128, 32], F32)
        S4 = sb("S4", [128, 16], F32)
        W3 = sb("W3", [128, 4, 12], F32, nb=4)
        Mst = sb("Mst", [128, 4], F32)
        Mx = sb("Mx", [128, 4], F32)
        inv4 = sb("inv4", [128, 4], F32)
        neghalf = sb("neghalf", [128, 16], F32)
        l1 = sb("l1", [128, 4], F32)
        gq = sb("gq", [128, 8], F32)
        apr = sb("apr", [128, 4], F32)
        Eex = sb("Eex", [128, 4], F32)
        Rl = sb("Rl", [128, 4], F32)
        Rr = sb("Rr", [128, 4], F32)
        ms = sb("ms", [128, 4], F32)
        D3 = sb("D3", [128, 12], F32)
        X3 = sb("X3", [128, 12], F32)
        dd = sb("dd", [128, 4], F32)
        rd = sb("rd", [128, 4], F32)
        ss4 = sb("ss4", [128, 4], F32)
        t4 = sb("t4", [128, 4], F32)
        sc4 = sb("sc4", [128, 4], F32)
        den8 = sb("den8", [128, 8], F32)
        rden8 = sb("rden8", [128, 8], F32)

        b_scfm, b_scg, b_scu, b_scd = Buf("scfm"), Buf("scg"), Buf("scu"), Buf("scd")
        b_out = Buf("out")
        b_c = Buf("consts")

        gbias = lambda: par[:, 0:8]
        sinks_ap = lambda: par[:, 8:16]
        gqk_ap = lambda: par[:, 16:656]
        gm_ap = lambda: par[:, 656:1168]

        def cload(dst, src):
            P.dma(sp, mk("dma_start", out=dst, in_=src), b_c, writes=[b_c], deps=False)
        cload(ident[:], id_d)
        cload(masks[:], mk_d)
        cload(negm[:], nm_d)
        cload(tri[:], tri_d)
        cload(g12[:], g12_d)
        cload(cw[:], cw_d)
        cload(cs[:], cs_d)
        cload(par[:], bass.AP(par_d.tensor, 0, [[0, 128], [1, NPAR]]))

        def load_x(m):
            for s in range(4):
                r0 = m * T + s * 128
                P.dma(sp, mk("dma_start", out=xs[m % 2][s][:], in_=x_d[r0:r0 + 128, :]),
                      xs[m % 2][s].b, writes=[xs[m % 2][s].b])

        load_x(0)
        for k in range(8):
            P.dma(pool, mk("dma_start", out=WIT[:, k, :], in_=wtm_d[k * 128:(k + 1) * 128, :]),
                  WIT.b, writes=[WIT.b], deps=False)
        for k in range(8):
            P.dma(pool, mk("dma_start",
                out=scfm_d[:, :, k * 128:(k + 1) * 128].rearrange("b p c -> p b c"),
                in_=wfm_d[k * 128:(k + 1) * 128, :].rearrange("p (b c) -> p b c", c=128)),
                b_scfm, writes=[b_scfm], deps=False)
        for k in range(8):
            P.dma(pool, mk("dma_start", out=WO[:, k, :], in_=wo_d[k * 128:(k + 1) * 128, :]),
                  WO.b, writes=[WO.b], deps=False)
        for (src, dst, bb) in ((wg_d, scg_d, b_scg), (wu_d, scu_d, b_scu)):
            for k in range(8):
                P.dma(pool, mk("dma_start",
                    out=dst[:, :, k * 128:(k + 1) * 128].rearrange("b p c -> p b c"),
                    in_=src[k * 128:(k + 1) * 128, :].rearrange("p (b c) -> p b c", c=128)),
                    bb, writes=[bb], deps=False)
        for f in range(NFF):
            P.dma(pool, mk("dma_start", out=scd_d[f * 128:(f + 1) * 128, :],
                                                  in_=wd_d[f * 128:(f + 1) * 128, :]),
                  b_scd, writes=[b_scd], deps=False)

        P.op(dve, mk("tensor_scalar", out=g12[:], in0=g12[:], scalar1=32.0, scalar2=None, op0=ALU.mult),
             reads=[b_c], writes=[b_c])
        P.op(dve, mk("tensor_scalar", out=par[:, 16:656], in0=par[:, 16:656], scalar1=8.0, scalar2=None, op0=ALU.mult),
             reads=[b_c], writes=[b_c])
        P.op(dve, mk("memset", C32[:], 0.0), writes=[C32.b])
        P.op(dve, mk("memset", Mst[:], 1.0), writes=[Mst.b])
        P.op(pool, mk("memset", neghalf[:], -0.5), writes=[neghalf.b])
        P.op(dve, mk("memset", halo[:], 0.0), writes=halo.b)
        for i in range(3):
            P.op(dve, mk("memset", Vaug[i][:], 1.0), writes=[Vaug[i].b])
        P.op(act, mk("activation", out=esink[:], in_=sinks_ap(), func=AF.Exp), reads=[b_c], writes=[esink.b])

        ring_i = [0]
        dring_i = [0]

        fring_i = [0]

        def ring_load(src_ap, srcbuf, fm=False):
            if fm:
                slot = fring[fring_i[0] % 2]
                fring_i[0] += 1
            else:
                slot = ring[ring_i[0] % NR]
                ring_i[0] += 1
            P.dma(sp, mk("dma_start", out=slot[:], in_=src_ap.rearrange("p (k c) -> p k c", c=128)),
                  slot.b, reads=[srcbuf], writes=[slot.b])
            return slot

        def dring_load(f0, half):
            slot = dring[dring_i[0] % ND]
            dring_i[0] += 1
            P.dma(sp, mk("dma_start",
                out=slot[:], in_=scd_d[f0 * 128:(f0 + 2) * 128, half * 512:(half + 1) * 512]
                .rearrange("(f p) c -> p f c", p=128)),
                slot.b, reads=[b_scd], writes=[slot.b])
            return slot

        def rsqrt_pool(dst, src, n, scale):
            P.op(pool, mk("tensor_scalar", out=dst[:, 0:n], in0=src[:, 0:n], scalar1=EPS / scale, scalar2=None,
                          op0=ALU.add), reads=[src.b], writes=[dst.b])
            P.op(pool, mk("tensor_tensor", out=dst[:, 0:n], in0=dst[:, 0:n], in1=neghalf[:, 0:n], op=ALU.pow),
                 reads=[dst.b, neghalf.b], writes=[dst.b])

        def rms_A(xt, par_i):
            u = ubf[par_i]
            P.op(act, mk("activation", out=u[:], in_=xt[:], func=AF.Square, accum_out=ssq[:]),
                 reads=[xt.b], writes=[u.b, ssq.b])
            rsqrt_pool(rstd, ssq, 1, 1.0 / D_MODEL)
            P.op(act, mk("activation", out=u[:], in_=xt[:], func=AF.Copy, scale=rstd[:]),
                 reads=[xt.b, rstd.b], writes=[u.b])

        def rms_B(gcol, dstT, s, par_i):
            u = ubf[par_i]
            bk = nb_()
            bv = bk[:].bitcast(BF)
            for k in range(8):
                P.op(pe, mk("transpose", out=bv[:, k * 128:(k + 1) * 128], in_=u[:, k * 128:(k + 1) * 128],
                                                    identity=ident[:]),
                     reads=[u.b, b_c], writes=[bk.b])
            P.op(dve, mk("tensor_tensor",
                out=dstT[:, :, s * 128:(s + 1) * 128], in0=bv.rearrange("p (k t) -> p k t", k=8),
                in1=g12[:, gcol:gcol + 8].unsqueeze(2).to_broadcast([128, 8, 128]), op=ALU.mult),
                reads=[bk.b, b_c], writes=[dstT.b[s]])

        def transpose_to(src, nblk, dst_ap, dst_bufs, extra=None):
            bk = nb_()
            bv = bk[:].bitcast(BF)
            for j in range(nblk):
                P.op(pe, mk("transpose", out=bv[:, j * 128:(j + 1) * 128], in_=src[:, j * 128:(j + 1) * 128],
                                                    identity=ident[:]),
                     reads=[src.b, b_c], writes=[bk.b])
            return bk, bv

        def mixer(m):
            xsm = xs[m % 2]
            if m > 0:
                load_x(m)
            rms_A(xsm[0], 0)
            yield 'A'
            for s in range(4):
                if s + 1 < 4:
                    rms_A(xsm[s + 1], (s + 1) % 2)
                    yield 'A'
                rms_B(0, uT, s, s % 2)
                yield 'A'
            bGa = nb_()
            for s in range(4):
                for k in range(8):
                    P.op(pe, mk("matmul", out=bGa[:, s * 8:(s + 1) * 8], lhsT=uT[:, k, s * 128:(s + 1) * 128],
                                rhs=WIT[:, k, 1792:1800], start=(k == 0), stop=(k == 7)),
                         reads=[uT.b[s], WIT.b], writes=[bGa.b])
            v48 = lambda t: t[:, 0:32].rearrange("p (s g) -> p s g", s=4)
            v44 = lambda t: t[:, 0:16].rearrange("p (s g) -> p s g", s=4)
            P.op(dve, mk("tensor_tensor", out=v48(gsb4), in0=v48(bGa), in1=gbias().unsqueeze(1).to_broadcast([128, 4, 8]),
                         op=ALU.add), reads=[bGa.b, b_c], writes=[gsb4.b])
            P.op(act, mk("activation", out=v44(l14), in_=v48(gsb4)[:, :, 4:8], func=AF.Exp, scale=-1.0),
                 reads=[gsb4.b], writes=[l14.b])
            P.op(act, mk("activation", out=l14[:], in_=l14[:], func=AF.Ln, bias=1.0), reads=[l14.b], writes=[l14.b])
            yield 'A'
            yield 'A'
            bGb = nb_()
            for s in range(4):
                P.op(pe, mk("matmul", out=bGb[:, s * 8:s * 8 + 4], lhsT=tri[:, 0:128], rhs=l14[:, s * 4:(s + 1) * 4],
                            start=True, stop=True), reads=[l14.b, b_c], writes=[bGb.b])
                P.op(pe, mk("matmul", out=bGb[:, s * 8 + 4:s * 8 + 8], lhsT=tri[:, 128:256], rhs=l14[:, s * 4:(s + 1) * 4],
                            start=True, stop=True), reads=[l14.b, b_c], writes=[bGb.b])
            P.op(dve, mk("tensor_copy", out=gq4[:], in_=bGb[:, 0:32]), reads=[bGb.b], writes=[gq4.b])
            P.op(dve, mk("tensor_tensor", out=v44(apr4), in0=v48(gsb4)[:, :, 0:4], in1=v48(gq4)[:, :, 0:4], op=ALU.add),
                 reads=[gsb4.b, gq4.b], writes=[apr4.b])
            P.op(act, mk("activation", out=E4[:], in_=apr4[:], func=AF.Exp), reads=[apr4.b], writes=[E4.b])
            P.op(act, mk("activation", out=v48(XG)[:, :, 0:4], in_=v48(gq4)[:, :, 0:4], func=AF.Exp),
                 reads=[gq4.b], writes=[XG.b])
            P.op(act, mk("activation", out=v48(XG)[:, :, 4:8], in_=v48(gq4)[:, :, 4:8], func=AF.Exp, scale=-1.0),
                 reads=[gq4.b], writes=[XG.b])
            yield 'A'
            yield 'A'
            bGc = nb_()
            for s in range(4):
                P.op(pe, mk("matmul", out=bGc[:, s * 4:(s + 1) * 4], lhsT=tri[:, 128:256], rhs=E4[:, s * 4:(s + 1) * 4],
                            start=True, stop=True), reads=[E4.b, b_c], writes=[bGc.b])
            P.op(dve, mk("tensor_copy", out=S4[:], in_=bGc[:, 0:16]), reads=[bGc.b], writes=[S4.b])
            for s in range(4):
                c4 = slice(s * 4, (s + 1) * 4)
                P.op(dve, mk("tensor_tensor", out=Mx[:], in0=S4[:, c4], in1=Mst[:], op=ALU.max),
                     reads=[S4.b, Mst.b], writes=[Mx.b])
                P.op(dve, mk("reciprocal", out=inv4[:], in_=Mx[:]), reads=[Mx.b], writes=[inv4.b])
                P.op(dve, mk("tensor_tensor", out=W3[:, s, 0:4], in0=E4[:, c4], in1=inv4[:], op=ALU.mult),
                     reads=[E4.b, inv4.b], writes=[W3.b[s]])
                P.op(dve, mk("tensor_tensor", out=W3[:, s, 4:8], in0=Mst[:], in1=inv4[:], op=ALU.mult),
                     reads=[Mst.b, inv4.b], writes=[W3.b[s]])
                P.op(dve, mk("scalar_tensor_tensor", out=W3[:, s, 8:12], in0=XG[:, s * 8:s * 8 + 4], scalar=2.0, in1=inv4[:],
                             op0=ALU.mult, op1=ALU.mult), reads=[XG.b, inv4.b], writes=[W3.b[s]])
                P.op(dve, mk("tensor_tensor", out=Mst[:], in0=Mx[:], in1=XG[:, s * 8 + 4:s * 8 + 8], op=ALU.mult),
                     reads=[Mx.b, XG.b], writes=[Mst.b])
            yield 'A'
            slots = {}
            for blk in range(2):
                slots[blk] = ring_load(scfm_d[blk], b_scfm, fm=True)
            for blk in range(8):
                slot = slots[blk]
                bk = nb_()
                for k in range(8):
                    P.op(pe, mk("matmul", out=bk[:], lhsT=slot[:, k, :], rhs=uT[:, k, :],
                                                                      start=(k == 0), stop=(k == 7)),
                         reads=[slot.b] + uT.b, writes=[bk.b])
                pr = pre[blk % 2]
                y = yc[blk % 2]
                P.op(act, mk("activation", out=pr[:, 3:T + 3], in_=bk[:], func=AF.Copy),
                     reads=[bk.b], writes=[pr.b])
                P.op(dve, mk("tensor_copy", out=pr[:, 0:3], in_=halo[:, blk, :]),
                     reads=[halo.b[blk]], writes=[pr.b])
                P.op(dve, mk("tensor_scalar",
                    out=y[:], in0=pr[:, 0:T], scalar1=cw[:, blk * 4:blk * 4 + 1], scalar2=None, op0=ALU.mult),
                    reads=[pr.b, b_c], writes=[y.b])
                for j in range(1, 4):
                    P.op(dve, mk("scalar_tensor_tensor",
                        out=y[:], in0=pr[:, j:T + j], scalar=cw[:, blk * 4 + j:blk * 4 + j + 1], in1=y[:],
                        op0=ALU.mult, op1=ALU.add),
                        reads=[pr.b, b_c, y.b], writes=[y.b])
                P.op(dve, mk("tensor_copy", out=halo[:, blk, :], in_=pr[:, T:T + 3]),
                     reads=[pr.b], writes=[halo.b[blk]])
                P.op(act, mk("activation", out=pr[:, 0:T], in_=y[:], func=AF.Tanh, scale=0.5),
                     reads=[y.b], writes=[pr.b])
                P.op(dve, mk("scalar_tensor_tensor", out=qkT[:, blk, :], in0=pr[:, 0:T], scalar=1.0, in1=y[:],
                             op0=ALU.add, op1=ALU.mult),
                     reads=[pr.b, y.b], writes=[qkT.b[blk]])
                if blk + 2 < 8:
                    slots[blk + 2] = ring_load(scfm_d[blk + 2], b_scfm, fm=True)
                yield 'A'

            def tm_proj(s, cols):
                sl = slice(s * 128, (s + 1) * 128)
                tb = []
                for (c0, c1) in cols:
                    bk = nb_()
                    tb.append(bk)
                    for k in range(8):
                        P.op(pe, mk("matmul",
                            out=bk[:, 0:c1 - c0], lhsT=uT[:, k, sl], rhs=WIT[:, k, c0:c1],
                            start=(k == 0), stop=(k == 7)),
                            reads=[uT.b[s], WIT.b], writes=[bk.b])
                return tb

            def mlstm_chain(s):
                gt = m * 4 + s
                sl = slice(s * 128, (s + 1) * 128)
                Bv, Bo = tm_proj(s, [(0, 512), (512, 1024)])
                P.op(act, mk("activation", out=v_sb[:], in_=Bv[:], func=AF.Copy), reads=[Bv.b], writes=[v_sb.b])
                P.op(act, mk("activation", out=og[:], in_=Bo[:], func=AF.Exp, scale=-1.0), reads=[Bo.b], writes=[og.b])
                P.op(dve, mk("tensor_scalar", out=og[:], in0=og[:], scalar1=1.0, scalar2=None, op0=ALU.add),
                     reads=[og.b], writes=[og.b])
                P.op(dve, mk("reciprocal", out=og[:], in_=og[:]), reads=[og.b], writes=[og.b])
                P.op(dve, mk("tensor_tensor", out=og[:], in0=og[:], in1=gm_ap(), op=ALU.mult),
                     reads=[og.b, b_c], writes=[og.b])

                yield
                kscale = 0.5 * 128.0 ** -0.5
                P.op(dve, mk("scalar_tensor_tensor",
                    out=vt[:, :, 0:128], in0=v_sb[:].rearrange("p (h d) -> p h d", h=4), scalar=kscale,
                    in1=W3[:, s, 0:4].unsqueeze(2).to_broadcast([128, 4, 128]), op0=ALU.mult, op1=ALU.mult),
                    reads=[v_sb.b, W3.b[s]], writes=[vt.b])
                P.op(dve, mk("tensor_scalar", out=vt[:, :, 128:129], in0=W3[:, s, 0:4].unsqueeze(2), scalar1=kscale,
                                                    scalar2=None, op0=ALU.mult),
                     reads=[W3.b[s]], writes=[vt.b])
                P.op(dve, mk("tensor_tensor", out=C32[:], in0=C32[:],
                                                    in1=W3[:, s, 4:8].unsqueeze(2).to_broadcast([128, 4, 129]), op=ALU.mult),
                     reads=[C32.b, W3.b[s]], writes=[C32.b])
                P.op(act, mk("activation", out=Cbf[:], in_=C32[:], func=AF.Copy), reads=[C32.b], writes=[Cbf.b])

                yield
                bKt = nb_()
                bKv = bKt[:].bitcast(BF)
                for h in range(4):
                    P.op(pe, mk("transpose", out=bKv[:, h * 128:(h + 1) * 128], in_=qkT[:, 4 + h, sl],
                                                        identity=ident[:]),
                         reads=[qkT.b[4 + h], b_c], writes=[bKt.b])
                P.op(act, mk("activation", out=k_tm[:], in_=bKv[:, 0:512], func=AF.Copy), reads=[bKt.b], writes=[k_tm.b])
                bS = nb_()
                for h in range(4):
                    P.op(pe, mk("matmul", out=bS[:, h * 128:(h + 1) * 128], lhsT=qkT[:, 4 + h, sl],
                                                     rhs=qkT[:, h, sl], start=True, stop=True),
                         reads=[qkT.b[4 + h], qkT.b[h]], writes=[bS.b])
                P.op(dve, mk("tensor_tensor",
                    out=Sm[:], in0=bS[:].rearrange("p (h l) -> p h l", h=4),
                    in1=masks[:, 0:128].unsqueeze(1).to_broadcast([128, 4, 128]), op=ALU.mult),
                    reads=[bS.b, b_c], writes=[Sm.b])
                yield
                yield
                yield
                bN = [nb_(), nb_()]
                for h in range(4):
                    bk = bN[h // 2]
                    o0 = (h % 2) * 129
                    P.op(pe, mk("matmul", out=bk[:, o0:o0 + 129], lhsT=Sm[:, h, :], rhs=vt[:, h, :],
                                                                   start=True, stop=False),
                         reads=[Sm.b, vt.b], writes=[bk.b])
                    P.op(pe, mk("matmul", out=bk[:, o0:o0 + 129], lhsT=qkT[:, h, sl], rhs=Cbf[:, h, :],
                                                                   start=False, stop=True),
                         reads=[qkT.b[h], Cbf.b], writes=[bk.b])
                bD = [nb_(), nb_()]
                for h in range(4):
                    bk = bD[h // 2]
                    o0 = (h % 2) * 129
                    P.op(pe, mk("matmul", out=bk[:, o0:o0 + 129], lhsT=k_tm[:, h * 128:(h + 1) * 128],
                                                                   rhs=vt[:, h, :], start=True, stop=True),
                         reads=[k_tm.b, vt.b], writes=[bk.b])
                for i in range(2):
                    P.op(dve, mk("tensor_tensor",
                        out=C32[:, 2 * i:2 * i + 2, :], in0=C32[:, 2 * i:2 * i + 2, :],
                        in1=bD[i][:, 0:258].rearrange("p (h e) -> p h e", h=2), op=ALU.add),
                        reads=[C32.b, bD[i].b], writes=[C32.b])
                den4 = dd
                for i in range(2):
                    P.op(act, mk("activation", out=hmf[:, 256 * i:256 * i + 256].rearrange("p (h e) -> p h e", h=2),
                                 in_=bN[i][:, 0:258].rearrange("p (h e) -> p h e", h=2)[:, :, 0:128], func=AF.Copy),
                         reads=[bN[i].b], writes=[hmf.b])
                    P.op(dve, mk("tensor_copy", out=t4[:, 2 * i:2 * i + 2].unsqueeze(2),
                                 in_=bN[i][:, 0:258].rearrange("p (h e) -> p h e", h=2)[:, :, 128:129]),
                         reads=[bN[i].b], writes=[t4.b])
                yield
                P.op(dve, mk("tensor_tensor", out=dd[:], in0=t4[:], in1=W3[:, s, 8:12], op=ALU.max),
                     reads=[t4.b, W3.b[s]], writes=[dd.b])
                P.op(dve, mk("scalar_tensor_tensor", out=dd[:], in0=t4[:], scalar=-1.0, in1=dd[:], op0=ALU.mult, op1=ALU.max),
                     reads=[t4.b, dd.b], writes=[dd.b])
                for h in range(4):
                    P.op(act, mk("activation", out=hm[:, 0:128], in_=hmf[:, h * 128:(h + 1) * 128],
                                 func=AF.Square, accum_out=ss4[:, h:h + 1]),
                         reads=[hmf.b], writes=[hm.b, ss4.b])
                P.op(dve, mk("scalar_tensor_tensor", out=rd[:], in0=dd[:], scalar=EPS, in1=dd[:], op0=ALU.mult, op1=ALU.mult),
                     reads=[dd.b], writes=[rd.b])
                P.op(dve, mk("scalar_tensor_tensor", out=sc4[:], in0=ss4[:], scalar=1.0 / 128, in1=rd[:], op0=ALU.mult, op1=ALU.add),
                     reads=[ss4.b, rd.b], writes=[sc4.b])
                P.op(pool, mk("tensor_tensor", out=sc4[:], in0=sc4[:], in1=neghalf[:, 0:4], op=ALU.pow),
                     reads=[sc4.b, neghalf.b], writes=[sc4.b])
                yield
                for h in range(4):
                    hs = slice(h * 128, (h + 1) * 128)
                    P.op(dve, mk("scalar_tensor_tensor", out=hm[:, hs], in0=hmf[:, hs], scalar=sc4[:, h:h + 1], in1=og[:, hs],
                                 op0=ALU.mult, op1=ALU.mult),
                         reads=[hmf.b, sc4.b, og.b], writes=[hm.b])
                yield
                yield
                bk, bv = transpose_to(hm, 4, None, None)
                P.op(act, mk("activation", out=hcatT[:, 0:4, (s % 2) * 128:(s % 2 + 1) * 128], in_=bv[:, 0:512].rearrange("p (k t) -> p k t", k=4), func=AF.Copy),
                     reads=[bk.b], writes=[hcatT.b[s % 2]])
                yield

            def swa_front(s):
                gt = m * 4 + s
                QT = QT2[gt % 2]
                Bq, Bk = tm_proj(s, [(1024, 1536), (1536, 1792)])
                P.op(act, mk("activation", out=qk_sb[:, 0:512], in_=Bq[:], func=AF.Copy), reads=[Bq.b], writes=[qk_sb.b])
                P.op(act, mk("activation", out=qk_sb[:, 512:640], in_=Bk[:, 0:128], func=AF.Copy),
                     reads=[Bk.b], writes=[qk_sb.b])
                Va = Vaug[gt % 3]
                P.op(act, mk("activation", out=Va[:, :, 0:64],
                                                      in_=Bk[:, 128:256].rearrange("p (g d) -> p g d", g=2), func=AF.Copy),
                     reads=[Bk.b], writes=[Va.b])
                yield
                P.op(dve, mk("tensor_tensor", out=sq[:], in0=qk_sb[:], in1=qk_sb[:], op=ALU.mult),
                     reads=[qk_sb.b], writes=[sq.b])
                P.op(dve, mk("tensor_reduce", out=ssq10[:], in_=sq[:].rearrange("p (h d) -> p h d", h=10),
                                                    axis=AX.X, op=ALU.add),
                     reads=[sq.b], writes=[ssq10.b])
                rsqrt_pool(r10, ssq10, 10, 1.0 / 64)
                P.op(dve, mk("tensor_tensor",
                    out=sq[:].rearrange("p (h d) -> p h d", h=10), in0=qk_sb[:].rearrange("p (h d) -> p h d", h=10),
                    in1=r10[:].unsqueeze(2).to_broadcast([128, 10, 64]), op=ALU.mult),
                    reads=[qk_sb.b, r10.b], writes=[sq.b])
                P.op(dve, mk("tensor_tensor", out=sq[:], in0=sq[:], in1=gqk_ap(), op=ALU.mult),
                     reads=[sq.b, b_c], writes=[sq.b])
                P.op(dve, mk("tensor_copy", out=qkn[:], in_=sq[:]), reads=[sq.b], writes=[qkn.b])
                sq3 = lambda: sq[:].rearrange("p (h d) -> p h d", h=10)
                qn3 = lambda: qkn[:].rearrange("p (h d) -> p h d", h=10)
                cosb = lambda: cs[:, gt * 16:gt * 16 + 8].unsqueeze(1).to_broadcast([128, 10, 8])
                sinb = lambda: cs[:, gt * 16 + 8:gt * 16 + 16].unsqueeze(1).to_broadcast([128, 10, 8])
                rt3 = lambda i: rt[:, i, :].rearrange("p (h d) -> p h d", h=10)
                P.op(dve, mk("tensor_tensor", out=rt3(0), in0=sq3()[:, :, 0:8], in1=cosb(), op=ALU.mult),
                     reads=[sq.b, b_c], writes=[rt.b])
                P.op(dve, mk("tensor_tensor", out=rt3(1), in0=sq3()[:, :, 8:16], in1=sinb(), op=ALU.mult),
                     reads=[sq.b, b_c], writes=[rt.b])
                P.op(dve, mk("tensor_tensor", out=rt3(2), in0=sq3()[:, :, 8:16], in1=cosb(), op=ALU.mult),
                     reads=[sq.b, b_c], writes=[rt.b])
                P.op(dve, mk("tensor_tensor", out=rt3(3), in0=sq3()[:, :, 0:8], in1=sinb(), op=ALU.mult),
                     reads=[sq.b, b_c], writes=[rt.b])
                P.op(dve, mk("tensor_tensor", out=qn3()[:, :, 0:8], in0=rt3(0), in1=rt3(1), op=ALU.subtract),
                     reads=[rt.b], writes=[qkn.b])
                P.op(dve, mk("tensor_tensor", out=qn3()[:, :, 8:16], in0=rt3(2), in1=rt3(3), op=ALU.add),
                     reads=[rt.b], writes=[qkn.b])
                yield
                yield
                bk, bv = transpose_to(qkn, 5, None, None)
                KTc = KT[gt % 3]
                P.op(act, mk("activation", out=QT[:], in_=bv[:, 0:512].rearrange("p (k t) -> p k t", k=4), func=AF.Copy),
                     reads=[bk.b], writes=[QT.b])
                P.op(dve, mk("tensor_copy", out=KTc[:], in_=bv[:, 512:640]), reads=[bk.b], writes=[KTc.b])
                yield

            def swa_back(s):
                gt = m * 4 + s
                sl = slice(s * 128, (s + 1) * 128)
                QT = QT2[gt % 2]
                KTc, Va = KT[gt % 3], Vaug[gt % 3]
                blocks = [(KTc, Va, 0)] if gt == 0 else [(KT[(gt - 1) % 3], Vaug[(gt - 1) % 3], 128), (KTc, Va, 0)]
                for g in range(2):
                    ps_ = slice(g * 64, (g + 1) * 64)
                    for bi, (Kt, Vt, moff) in enumerate(blocks):
                        idx = g * 2 + bi
                        bSc = nb_()
                        P.op(pe, mk("matmul", out=bSc[:], lhsT=Kt[ps_, :], rhs=QT[ps_, :, :],
                                                                            start=True, stop=False),
                             reads=[Kt.b, QT.b], writes=[bSc.b])
                        P.op(pe, mk("matmul", out=bSc[:], lhsT=ident[:],
                                    rhs=negm[:, moff:moff + 128].unsqueeze(1).to_broadcast([128, 4, 128]),
                                    start=False, stop=True),
                             reads=[b_c], writes=[bSc.b])
                        P.op(act, mk("activation", out=PT[:, idx, :], in_=bSc[:], func=AF.Exp, scale=0.125),
                             reads=[bSc.b], writes=[PT.b[idx]])
                yield
                bO = [nb_(), nb_()]
                for h in range(8):
                    g, j = h // 4, h % 4
                    for bi, (Kt, Vt, moff) in enumerate(blocks):
                        idx = g * 2 + bi
                        P.op(pe, mk("matmul",
                            out=bO[g][:, j * 65:(j + 1) * 65], lhsT=PT[:, idx, j * 128:(j + 1) * 128], rhs=Vt[:, g, :],
                            start=(bi == 0), stop=(bi == len(blocks) - 1)),
                            reads=[PT.b[idx], Vt.b], writes=[bO[g].b])
                for g in range(2):
                    P.op(dve, mk("tensor_tensor",
                        out=den8[:, 4 * g:4 * g + 4].unsqueeze(2),
                        in0=bO[g][:, 0:260].rearrange("p (h e) -> p h e", h=4)[:, :, 64:65],
                        in1=esink[:, 4 * g:4 * g + 4].unsqueeze(2), op=ALU.add),
                        reads=[bO[g].b, esink.b], writes=[den8.b])
                P.op(dve, mk("reciprocal", out=rden8[:], in_=den8[:]), reads=[den8.b], writes=[rden8.b])
                for g in range(2):
                    P.op(dve, mk("tensor_tensor",
                        out=ha[:, 256 * g:256 * g + 256].rearrange("p (h d) -> p h d", h=4),
                        in0=bO[g][:, 0:260].rearrange("p (h e) -> p h e", h=4)[:, :, 0:64],
                        in1=rden8[:, 4 * g:4 * g + 4].unsqueeze(2).to_broadcast([128, 4, 64]), op=ALU.mult),
                        reads=[bO[g].b, rden8.b], writes=[ha.b])
                yield
                yield
                bk, bv = transpose_to(ha, 4, None, None)
                P.op(act, mk("activation", out=hcatT[:, 4:8, (s % 2) * 128:(s % 2 + 1) * 128], in_=bv[:, 0:512].rearrange("p (k t) -> p k t", k=4),
                                                      func=AF.Copy),
                     reads=[bk.b], writes=[hcatT.b[s % 2]])


            def join_chain(s):
                sl = slice(s * 128, (s + 1) * 128)
                xt = xsm[s]
                for half in range(2):
                    bk = nb_()
                    for k in range(8):
                        P.op(pe, mk("matmul",
                            out=bk[:], lhsT=hcatT[:, k, (s % 2) * 128:(s % 2 + 1) * 128], rhs=WO[:, k, half * 512:(half + 1) * 512],
                            start=(k == 0), stop=(k == 7)),
                            reads=[hcatT.b[s % 2], WO.b], writes=[bk.b])
                    P.op(dve, mk("tensor_tensor",
                        out=xt[:, half * 512:(half + 1) * 512], in0=bk[:], in1=xt[:, half * 512:(half + 1) * 512], op=ALU.add),
                        reads=[bk.b, xt.b], writes=[xt.b])
                yield
                rms_A(xt, s % 2)
                yield
                yield
                yield
                yield
                rms_B(8, u2T, s, s % 2)
                yield

            def rr(gens):
                gens = list(gens)
                while gens:
                    for g in list(gens):
                        try:
                            next(g)
                            yield 'B'
                        except StopIteration:
                            gens.remove(g)
            yield from rr([swa_front(0)])
            for s in range(4):
                yield from rr([mlstm_chain(s), swa_back(s)] + ([join_chain(s - 1)] if s > 0 else [])
                              + ([swa_front(s + 1)] if s + 1 < 4 else []))
            yield from rr([join_chain(3)])

        def ffn(m):
            xsm = xs[m % 2]
            ffn_slots = {}

            def ffn_fetch(f):
                if f < NFF and f not in ffn_slots:
                    ffn_slots[f] = (ring_load(scg_d[f], b_scg), ring_load(scu_d[f], b_scu))
            for f in range(NR // 2):
                ffn_fetch(f)
            dslots = {}

            def d_fetch(i):
                if i < 22 and i not in dslots:
                    dslots[i] = dring_load((i % 11) * 2, i // 11)
            for f in range(NFF):
                ffn_fetch(f)
                sg_, su_ = ffn_slots[f]
                bGt = nbf_()
                bUp = nbf_()
                for (slot, bk) in ((sg_, bGt), (su_, bUp)):
                    for k in range(8):
                        P.op(pe, mk("matmul", out=bk[:], lhsT=slot[:, k, :], rhs=u2T[:, k, :],
                                                                          start=(k == 0), stop=(k == 7)),
                             reads=[slot.b] + u2T.b, writes=[bk.b])
                    if bk is bGt:
                        yield 1.7
                ffn_fetch(f + NR // 2)
                sgt = sg[f % 2]
                P.op(act, mk("activation", out=sgt[:], in_=bGt[:], func=AF.Tanh, scale=0.5),
                     reads=[bGt.b], writes=[sgt.b])
                P.op(dve, mk("scalar_tensor_tensor", out=sgt[:], in0=sgt[:], scalar=1.0, in1=bGt[:],
                             op0=ALU.add, op1=ALU.mult),
                     reads=[sgt.b, bGt.b], writes=[sgt.b])
                P.op(dve, mk("scalar_tensor_tensor", out=actT[:, f, :], in0=sgt[:], scalar=0.5, in1=bUp[:],
                             op0=ALU.mult, op1=ALU.mult),
                     reads=[sgt.b, bUp.b], writes=[actT.b[f]])
                if f >= NFF - ND:
                    d_fetch(f - (NFF - ND))
                yield 3.5
            for half in range(2):
                bY = [nbf_() for _ in range(4)]
                for f in range(NFF):
                    i = half * 11 + f // 2
                    d_fetch(i)
                    slot = dslots[i]
                    for s in range(4):
                        P.op(pe, mk("matmul",
                            out=bY[s][:], lhsT=actT[:, f, s * 128:(s + 1) * 128], rhs=slot[:, f % 2, :],
                            start=(f == 0), stop=(f == NFF - 1)),
                            reads=[actT.b[f], slot.b], writes=[bY[s].b])
                    if f % 2 == 1:
                        d_fetch(i + ND)
                    yield 0.85
                for s in range(4):
                    P.op(dve, mk("tensor_tensor",
                        out=xsm[s][:, half * 512:(half + 1) * 512], in0=bY[s][:], in1=xsm[s][:, half * 512:(half + 1) * 512],
                        op=ALU.add),
                        reads=[bY[s].b, xsm[s].b], writes=[xsm[s].b])
            for s in range(4):
                r0 = m * T + s * 128
                P.dma(sp, mk("dma_start", out=out_d[r0:r0 + 128, :], in_=xsm[s][:]),
                      xsm[s].b, reads=[xsm[s].b], writes=[])
            yield 0.01

        def drive(*gens):
            gens = list(gens)
            while gens:
                for g in list(gens):
                    try:
                        next(g)
                    except StopIteration:
                        gens.remove(g)

        drive(mixer(0))
        for m in range(NM):
            if m + 1 < NM:
                drive(ffn(m), mixer(m + 1))
            else:
                drive(ffn(m))
        P.wait_all(sp, [x.b for xx in xs for x in xx])
        P.emit()
    return nc


def _host_layout(inputs, NT):
    f32 = np.float32
    w_in = np.asarray(inputs["w_in"], f32)[0]
    qperm = np.concatenate([np.arange(h * 64, (h + 1) * 64) for h in (0, 4, 1, 5, 2, 6, 3, 7)])
    cols_tm = np.concatenate([
        np.arange(1024, 1536), np.arange(1536, 2048), 2056 + qperm, np.arange(2568, 2696),
        np.arange(2696, 2824), np.arange(2048, 2056)])
    w_in_tm = np.ascontiguousarray(w_in[:, cols_tm])
    w_in_fm = np.ascontiguousarray(w_in[:, 0:1024])
    params = np.concatenate([
        np.asarray(inputs["igate_b"], f32)[0], np.asarray(inputs["fgate_b"], f32)[0],
        np.asarray(inputs["sinks"], f32)[0],
        np.tile(np.asarray(inputs["q_norm_g"], f32)[0], 8), np.tile(np.asarray(inputs["k_norm_g"], f32)[0], 2),
        np.asarray(inputs["mlstm_norm_g"], f32)[0]]).reshape(1, NPAR)
    g12 = np.ascontiguousarray(np.concatenate([
        np.asarray(inputs["norm1_g"], f32)[0].reshape(8, 128).T,
        np.asarray(inputs["norm2_g"], f32)[0].reshape(8, 128).T], axis=1))
    convw = np.ascontiguousarray(np.asarray(inputs["conv_w"], f32)[0].reshape(4, 8, 128).transpose(2, 1, 0).reshape(128, 32))
    pos = np.arange(NT, dtype=f32)
    inv_freq = (f32(500000.0) ** (-np.arange(0, 16, 2, dtype=f32) / f32(16))).astype(f32)
    ang = (pos[:, None] * inv_freq[None, :]).astype(f32)
    cs_ = np.concatenate([np.cos(ang), np.sin(ang)], axis=1).astype(f32)
    ropecs = np.ascontiguousarray(cs_.reshape(NT // 128, 128, 16).transpose(1, 0, 2).reshape(128, -1))
    tri_le = np.triu(np.ones((128, 128), f32))
    masks = np.concatenate([tri_le, 1.0 - tri_le], axis=1).astype(ml_dtypes.bfloat16)
    negmask = ((np.concatenate([tri_le, 1.0 - tri_le], axis=1) - 1.0) * 30000.0).astype(ml_dtypes.bfloat16)
    tri = np.concatenate([tri_le, np.ones((128, 128), f32)], axis=1)
    ident = np.eye(128, dtype=f32).astype(ml_dtypes.bfloat16)
    return dict(
        w_in_tm=w_in_tm, w_in_fm=w_in_fm, w_out=np.asarray(inputs["w_out"], f32)[0],
        w_gate=np.asarray(inputs["w_gate"], f32)[0], w_up=np.asarray(inputs["w_up"], f32)[0],
        w_down=np.asarray(inputs["w_down"], f32)[0], params=params, g12=g12, convw=convw, ropecs=ropecs,
        ident=ident, masks=masks, negmask=negmask, tri=tri)


def run(inputs, n_cores=8, NT=4096):
    x = np.asarray(inputs["x"], np.float32)
    shared = _host_layout(inputs, NT)
    nc = build_nc(NT)
    in_maps = [dict(shared, x=np.ascontiguousarray(x[b, :NT])) for b in range(n_cores)]
    res = run_bass_kernel_spmd(nc, in_maps, core_ids=list(range(n_cores)))
    return np.stack([np.asarray(r["out"], np.float32) for r in res.results], axis=0)


def kernel(**inputs):
    return run(inputs, n_cores=8, NT=4096)
```
